# Optimizing a Trainium2 kernel written in Bass

```python
import math
import jax, jax.numpy as jnp
from jax import lax
import numpy as np


D_MODEL = 2048
BATCH = 1
SEQ = 8192
DEPTH = 1
DEC_BATCH = 128
DEC_SEQ = 4
PAST_LEN = 2048
PAGE_SIZE = 128

SSM_WIDTH = D_MODEL // 2
SSM_GROUP = 16
SSM_GROUPS = SSM_WIDTH // SSM_GROUP
SSM_STATE = 64
DT_MIN = 0.001
DT_MAX = 0.1
HEAD_DIM = 128
DILATION_PATTERNS = ((128, 1), (512, 4), (2048, 16))
N_PATTERNS = len(DILATION_PATTERNS)
HEADS_PER_GROUP = 4
N_HEADS = N_PATTERNS * HEADS_PER_GROUP
ATTN_WIDTH = N_HEADS * HEAD_DIM
ROT_DIM = HEAD_DIM // 4
ROPE_THETA = 500000.0
OFF_Q = SSM_WIDTH
OFF_K = OFF_Q + ATTN_WIDTH
OFF_V = OFF_K + ATTN_WIDTH
OFF_G = OFF_V + ATTN_WIDTH
IN_COLS = OFF_G + 2 * D_MODEL
N_EXPERT_GROUPS = 4
EXPERTS_PER_GROUP = 4
N_EXPERTS = N_EXPERT_GROUPS * EXPERTS_PER_GROUP
EXPERT_TOP_K = 2
EXPERT_FF = D_MODEL // 4
EPS = 1e-6
F32 = jnp.float32

kernel_name = 'hybrid_s5_dilated_attn_hmoe_step'


def rms_norm(x, g):
    xf = x.astype(F32)
    y = xf * lax.rsqrt(jnp.mean(xf * xf, axis=-1, keepdims=True) + EPS)
    return (y * g.astype(F32)).astype(x.dtype)


def rope_partial(x, pos):
    half = ROT_DIM // 2
    inv_freq = ROPE_THETA ** (-jnp.arange(half, dtype=F32) / half)
    ang = pos.astype(F32)[:, None] * inv_freq[None, :]
    cos = jnp.cos(ang)[None, :, None, :]
    sin = jnp.sin(ang)[None, :, None, :]
    xf = x.astype(F32)
    x1 = xf[..., :half]
    x2 = xf[..., half:ROT_DIM]
    out = jnp.concatenate([x1 * cos - x2 * sin, x2 * cos + x1 * sin, xf[..., ROT_DIM:]], axis=-1)
    return out.astype(x.dtype)


def _complex_combine(e1, e2):
    a1r, a1i, b1r, b1i = e1
    a2r, a2i, b2r, b2i = e2
    return (a2r * a1r - a2i * a1i,
            a2r * a1i + a2i * a1r,
            a2r * b1r - a2i * b1i + b2r,
            a2r * b1i + a2i * b1r + b2i)


def s5_branch(u, s0_re, s0_im, a_re, a_im, log_dt, b_re, b_im, c_re, c_im, d_skip):
    Bsz, T, _ = u.shape
    a_re = a_re.astype(F32)
    a_im = a_im.astype(F32)
    dt = jnp.exp(log_dt.astype(F32))[:, None]
    mag = jnp.exp(a_re * dt)
    abar_re = mag * jnp.cos(a_im * dt)
    abar_im = mag * jnp.sin(a_im * dt)
    nr = abar_re - 1.0
    ni = abar_im
    den = a_re * a_re + a_im * a_im
    z_re = (nr * a_re + ni * a_im) / den
    z_im = (ni * a_re - nr * a_im) / den
    b_re = b_re.astype(F32)
    b_im = b_im.astype(F32)
    bbar_re = z_re[..., None] * b_re - z_im[..., None] * b_im
    bbar_im = z_re[..., None] * b_im + z_im[..., None] * b_re
    ug = u.astype(F32).reshape(Bsz, T, SSM_GROUPS, SSM_GROUP)
    bu_re = jnp.einsum('btgn,gpn->btgp', ug, bbar_re)
    bu_im = jnp.einsum('btgn,gpn->btgp', ug, bbar_im)
    ar = jnp.broadcast_to(abar_re, bu_re.shape)
    ai = jnp.broadcast_to(abar_im, bu_re.shape)
    acum_re, acum_im, st_re, st_im = lax.associative_scan(_complex_combine, (ar, ai, bu_re, bu_im), axis=1)
    s0r = s0_re.astype(F32)[:, None]
    s0i = s0_im.astype(F32)[:, None]
    s_re = acum_re * s0r - acum_im * s0i + st_re
    s_im = acum_re * s0i + acum_im * s0r + st_im
    y = (jnp.einsum('gnp,btgp->btgn', c_re.astype(F32), s_re)
         - jnp.einsum('gnp,btgp->btgn', c_im.astype(F32), s_im))
    y = y.reshape(Bsz, T, SSM_WIDTH) + d_skip.astype(F32) * u.astype(F32)
    return y.astype(u.dtype), s_re[:, -1].astype(u.dtype), s_im[:, -1].astype(u.dtype)


def dilated_window_prompt(q, k, v, dilation, band):
    Bsz, T, H, Dh = q.shape
    M = T // dilation
    Mp = -(-M // band) * band
    nb = Mp // band

    def to_blocks(x):
        x = x.reshape(Bsz, M, dilation, H, Dh).transpose(0, 2, 1, 3, 4)
        x = jnp.pad(x, ((0, 0), (0, 0), (0, Mp - M), (0, 0), (0, 0)))
        return x.reshape(Bsz, dilation, nb, band, H, Dh)

    def with_prev(x):
        prev = jnp.pad(x[:, :, :-1], ((0, 0), (0, 0), (1, 0), (0, 0), (0, 0), (0, 0)))
        return jnp.concatenate([prev, x], axis=3)

    qb = to_blocks(q).astype(F32)
    kw = with_prev(to_blocks(k)).astype(F32)
    vw = with_prev(to_blocks(v)).astype(F32)
    s = jnp.einsum('brnqhd,brnkhd->brnhqk', qb, kw) * (Dh ** -0.5)
    qi = jnp.arange(band)[:, None]
    kj = jnp.arange(2 * band)[None, :]
    dist = qi + band - kj
    key_m = jnp.arange(nb)[:, None, None] * band + kj[None] - band
    valid = ((dist >= 0) & (dist <= band))[None] & (key_m >= 0)
    s = jnp.where(valid[None, None, :, None], s, -jnp.inf)
    mx = jnp.max(s, axis=-1, keepdims=True)
    p = jnp.exp(s - mx)
    l = jnp.sum(p, axis=-1, keepdims=True)
    o = jnp.einsum('brnhqk,brnkhd->brnqhd', p / l, vw)
    lse = (mx + jnp.log(l))[..., 0]
    o = o.reshape(Bsz, dilation, Mp, H, Dh)[:, :, :M].transpose(0, 2, 1, 3, 4).reshape(Bsz, T, H, Dh)
    lse = lse.transpose(0, 1, 2, 4, 3).reshape(Bsz, dilation, Mp, H)[:, :, :M]
    lse = lse.transpose(0, 2, 1, 3).reshape(Bsz, T, H)
    return o, lse


def dilated_window_sample(q, k, v, cache_kv, dilation, band):
    Wb = cache_kv.shape[1]
    S = q.shape[1]
    Dh = q.shape[-1]
    kf = jnp.concatenate([cache_kv[:, :, 0], k], axis=1)
    vf = jnp.concatenate([cache_kv[:, :, 1], v], axis=1)
    idx = Wb + jnp.arange(S)[:, None] - jnp.arange(band + 1)[None, :] * dilation
    valid = idx >= 0
    idx = jnp.maximum(idx, 0)
    kg = kf[:, idx].astype(F32)
    vg = vf[:, idx].astype(F32)
    s = jnp.einsum('nshd,nskhd->nshk', q.astype(F32), kg) * (Dh ** -0.5)
    s = jnp.where(valid[None, :, None, :], s, -jnp.inf)
    mx = jnp.max(s, axis=-1, keepdims=True)
    p = jnp.exp(s - mx)
    l = jnp.sum(p, axis=-1, keepdims=True)
    o = jnp.einsum('nshk,nskhd->nshd', p / l, vg)
    lse = (mx + jnp.log(l))[..., 0]
    return o, lse


def hier_moe(h, w_rg, b_rg, w_re, b_re, w_gu, w_down):
    Bsz, T, D = h.shape
    hf = h.reshape(-1, D)
    lg = (hf @ w_rg + b_rg).astype(F32)
    g_sel = jnp.argmax(lg, axis=-1)
    p_group = jnp.take_along_axis(jax.nn.softmax(lg, axis=-1), g_sel[:, None], axis=1)
    le = (hf @ w_re + b_re).astype(F32).reshape(-1, N_EXPERT_GROUPS, EXPERTS_PER_GROUP)
    le_sel = jnp.take_along_axis(le, g_sel[:, None, None], axis=1)[:, 0]
    top_v, top_i = lax.top_k(le_sel, EXPERT_TOP_K)
    w_top = jax.nn.softmax(top_v, axis=-1) * p_group
    e_idx = g_sel[:, None] * EXPERTS_PER_GROUP + top_i
    combine = jnp.sum(jax.nn.one_hot(e_idx, N_EXPERTS, dtype=F32) * w_top[..., None], axis=1)
    gu = jnp.einsum('nd,edf->nef', hf, w_gu)
    gate, up = jnp.split(gu, 2, axis=-1)
    act = jax.nn.silu(gate) * up * combine[..., None].astype(h.dtype)
    out = jnp.einsum('nef,efd->nd', act, w_down)
    return out.reshape(Bsz, T, D)


def _layer(x, c, pos, s0_re, s0_im, kv_caches, p):
    Bsz, T, _ = x.shape
    mod = jax.nn.silu(c) @ p['w_ada'] + p['b_ada']
    sh1, sc1, gt1, sh2, sc2, gt2 = [m[:, None, :] for m in jnp.split(mod, 6, axis=-1)]
    h = rms_norm(x, p['norm1_g']) * (1.0 + sc1) + sh1
    proj = h @ p['w_in']
    u, q, k, v, gates = jnp.split(proj, [OFF_Q, OFF_K, OFF_V, OFF_G], axis=-1)
    y, sT_re, sT_im = s5_branch(u, s0_re, s0_im, p['ssm_a_re'], p['ssm_a_im'], p['ssm_log_dt'],
                                p['ssm_b_re'], p['ssm_b_im'], p['ssm_c_re'], p['ssm_c_im'], p['ssm_d'])
    ga, gb = jnp.split(jax.nn.gelu(y) @ p['w_glu'], 2, axis=-1)
    branch_a = ga * jax.nn.sigmoid(gb)
    q = rope_partial(rms_norm(q.reshape(Bsz, T, N_HEADS, HEAD_DIM), p['q_norm_g']), pos)
    k = rope_partial(rms_norm(k.reshape(Bsz, T, N_HEADS, HEAD_DIM), p['k_norm_g']), pos)
    v = v.reshape(Bsz, T, N_HEADS, HEAD_DIM)
    outs, lses, new_kv = [], [], []
    for g, (window, dilation) in enumerate(DILATION_PATTERNS):
        hs = slice(g * HEADS_PER_GROUP, (g + 1) * HEADS_PER_GROUP)
        qg, kg, vg = q[:, :, hs], k[:, :, hs], v[:, :, hs]
        band = window // dilation
        if kv_caches is None:
            o, l = dilated_window_prompt(qg, kg, vg, dilation, band)
            new_kv.append(jnp.stack([kg, vg], axis=2)[:, -min(window, T):])
        else:
            o, l = dilated_window_sample(qg, kg, vg, kv_caches[g], dilation, band)
            new_kv.append(jnp.stack([kg, vg], axis=2))
        outs.append(o)
        lses.append(l)
    alpha = jax.nn.softmax(jnp.stack(lses, axis=0), axis=0)
    o = jnp.sum(alpha[..., None] * jnp.stack(outs, axis=0), axis=0)
    branch_b = o.reshape(Bsz, T, HEADS_PER_GROUP * HEAD_DIM).astype(x.dtype) @ p['w_attn_br']
    gate_a, gate_b = jnp.split(jax.nn.sigmoid(gates), 2, axis=-1)
    mixed = (gate_a * branch_a + gate_b * branch_b) @ p['w_out']
    x = x + gt1 * mixed
    h2 = rms_norm(x, p['norm2_g']) * (1.0 + sc2) + sh2
    x = x + gt2 * hier_moe(h2, p['w_router_group'], p['b_router_group'], p['w_router_expert'],
                           p['b_router_expert'], p['w_expert_gate_up'], p['w_expert_down'])
    return x, new_kv, sT_re, sT_im


def setup_inputs(seed: int = 0) -> dict:
    key = jax.random.key(seed)
    ks = jax.random.split(key, 40)

    def nrm(k, shape, scale):
        return jax.random.normal(k, shape, F32) * scale

    L = DEPTH
    wins = [min(w, PAST_LEN) for (w, _) in DILATION_PATTERNS]
    n_idx = jnp.arange(SSM_STATE, dtype=F32)
    return {
        'x_prompt': nrm(ks[0], (BATCH, SEQ, D_MODEL), 1.0),
        'x_sample': nrm(ks[1], (DEC_BATCH, DEC_SEQ, D_MODEL), 1.0),
        'cache_kv_w128': nrm(ks[2], (L, DEC_BATCH, wins[0], 2, HEADS_PER_GROUP, HEAD_DIM), 1.0),
        'cache_kv_w512': nrm(ks[3], (L, DEC_BATCH, wins[1], 2, HEADS_PER_GROUP, HEAD_DIM), 1.0),
        'cache_kv_w2048': nrm(ks[4], (L, DEC_BATCH, wins[2], 2, HEADS_PER_GROUP, HEAD_DIM), 1.0),
        'state_ssm_re': nrm(ks[5], (L, DEC_BATCH, SSM_GROUPS, SSM_STATE), 0.1),
        'state_ssm_im': nrm(ks[6], (L, DEC_BATCH, SSM_GROUPS, SSM_STATE), 0.1),
        'c_prompt': nrm(ks[7], (BATCH, D_MODEL), 1.0),
        'c_sample': nrm(ks[8], (DEC_BATCH, D_MODEL), 1.0),
        'w_ada': nrm(ks[9], (L, D_MODEL, 6 * D_MODEL), 0.5 * D_MODEL ** -0.5),
        'b_ada': nrm(ks[10], (L, 6 * D_MODEL), 0.01),
        'norm1_g': 1.0 + nrm(ks[11], (L, D_MODEL), 0.01),
        'norm2_g': 1.0 + nrm(ks[12], (L, D_MODEL), 0.01),
        'w_in': nrm(ks[13], (L, D_MODEL, IN_COLS), D_MODEL ** -0.5),
        'ssm_a_re': -0.5 + nrm(ks[14], (L, SSM_GROUPS, SSM_STATE), 0.01),
        'ssm_a_im': math.pi * n_idx + nrm(ks[15], (L, SSM_GROUPS, SSM_STATE), 0.01),
        'ssm_log_dt': jax.random.uniform(ks[16], (L, SSM_GROUPS), F32, math.log(DT_MIN), math.log(DT_MAX)),
        'ssm_b_re': nrm(ks[17], (L, SSM_GROUPS, SSM_STATE, SSM_GROUP), (2 * SSM_GROUP) ** -0.5),
        'ssm_b_im': nrm(ks[18], (L, SSM_GROUPS, SSM_STATE, SSM_GROUP), (2 * SSM_GROUP) ** -0.5),
        'ssm_c_re': nrm(ks[19], (L, SSM_GROUPS, SSM_GROUP, SSM_STATE), SSM_STATE ** -0.5),
        'ssm_c_im': nrm(ks[20], (L, SSM_GROUPS, SSM_GROUP, SSM_STATE), SSM_STATE ** -0.5),
        'ssm_d': nrm(ks[21], (L, SSM_WIDTH), 1.0),
        'w_glu': nrm(ks[22], (L, SSM_WIDTH, 2 * D_MODEL), SSM_WIDTH ** -0.5),
        'q_norm_g': 1.0 + nrm(ks[23], (L, HEAD_DIM), 0.01),
        'k_norm_g': 1.0 + nrm(ks[24], (L, HEAD_DIM), 0.01),
        'w_attn_br': nrm(ks[25], (L, HEADS_PER_GROUP * HEAD_DIM, D_MODEL), (HEADS_PER_GROUP * HEAD_DIM) ** -0.5),
        'w_out': nrm(ks[26], (L, D_MODEL, D_MODEL), D_MODEL ** -0.5),
        'w_router_group': nrm(ks[27], (L, D_MODEL, N_EXPERT_GROUPS), D_MODEL ** -0.5),
        'b_router_group': nrm(ks[28], (L, N_EXPERT_GROUPS), 0.01),
        'w_router_expert': nrm(ks[29], (L, D_MODEL, N_EXPERTS), D_MODEL ** -0.5),
        'b_router_expert': nrm(ks[30], (L, N_EXPERTS), 0.01),
        'w_expert_gate_up': nrm(ks[31], (L, N_EXPERTS, D_MODEL, 2 * EXPERT_FF), D_MODEL ** -0.5),
        'w_expert_down': nrm(ks[32], (L, N_EXPERTS, EXPERT_FF, D_MODEL), EXPERT_FF ** -0.5),
    }


def reference(x_prompt, x_sample, cache_kv_w128, cache_kv_w512, cache_kv_w2048, state_ssm_re, state_ssm_im,
              c_prompt, c_sample, w_ada, b_ada, norm1_g, norm2_g, w_in, ssm_a_re, ssm_a_im, ssm_log_dt,
              ssm_b_re, ssm_b_im, ssm_c_re, ssm_c_im, ssm_d, w_glu, q_norm_g, k_norm_g, w_attn_br, w_out,
              w_router_group, b_router_group, w_router_expert, b_router_expert, w_expert_gate_up, w_expert_down):
    pos_p = jnp.arange(x_prompt.shape[1], dtype=jnp.int32)
    pos_s = PAST_LEN + jnp.arange(x_sample.shape[1], dtype=jnp.int32)
    y_p, y_s = x_prompt, x_sample
    kvp = [[], [], []]
    kvs = [[], [], []]
    srp, sip, srs, sis = [], [], [], []
    for l in range(DEPTH):
        p = {'w_ada': w_ada[l], 'b_ada': b_ada[l], 'norm1_g': norm1_g[l], 'norm2_g': norm2_g[l],
             'w_in': w_in[l], 'ssm_a_re': ssm_a_re[l], 'ssm_a_im': ssm_a_im[l], 'ssm_log_dt': ssm_log_dt[l],
             'ssm_b_re': ssm_b_re[l], 'ssm_b_im': ssm_b_im[l], 'ssm_c_re': ssm_c_re[l], 'ssm_c_im': ssm_c_im[l],
             'ssm_d': ssm_d[l], 'w_glu': w_glu[l], 'q_norm_g': q_norm_g[l], 'k_norm_g': k_norm_g[l],
             'w_attn_br': w_attn_br[l], 'w_out': w_out[l], 'w_router_group': w_router_group[l],
             'b_router_group': b_router_group[l], 'w_router_expert': w_router_expert[l],
             'b_router_expert': b_router_expert[l], 'w_expert_gate_up': w_expert_gate_up[l],
             'w_expert_down': w_expert_down[l]}
        s0 = jnp.zeros((y_p.shape[0], SSM_GROUPS, SSM_STATE), y_p.dtype)
        y_p, nkv_p, sre_p, sim_p = _layer(y_p, c_prompt, pos_p, s0, s0, None, p)
        y_s, nkv_s, sre_s, sim_s = _layer(y_s, c_sample, pos_s, state_ssm_re[l], state_ssm_im[l],
                                          (cache_kv_w128[l], cache_kv_w512[l], cache_kv_w2048[l]), p)
        for g in range(N_PATTERNS):
            kvp[g].append(nkv_p[g])
            kvs[g].append(nkv_s[g])
        srp.append(sre_p)
        sip.append(sim_p)
        srs.append(sre_s)
        sis.append(sim_s)
    kv128_p = jnp.stack(kvp[0], axis=0)
    kv512_p = jnp.stack(kvp[1], axis=0)
    kv2048_p = jnp.stack(kvp[2], axis=0)
    ssm_re_p = jnp.stack(srp, axis=0)
    ssm_im_p = jnp.stack(sip, axis=0)
    kv128_s = jnp.stack(kvs[0], axis=0)
    kv512_s = jnp.stack(kvs[1], axis=0)
    kv2048_s = jnp.stack(kvs[2], axis=0)
    ssm_re_s = jnp.stack(srs, axis=0)
    ssm_im_s = jnp.stack(sis, axis=0)
    return (y_p, y_s, kv128_p, kv512_p, kv2048_p, ssm_re_p, ssm_im_p, kv128_s, kv512_s, kv2048_s, ssm_re_s, ssm_im_s)
```

```python
from contextlib import ExitStack
import concourse.bass as bass
import concourse.mybir as mybir

F32 = mybir.dt.float32
BF16 = mybir.dt.bfloat16
I32 = mybir.dt.int32
AF = mybir.ActivationFunctionType
ALU = mybir.AluOpType
AX = mybir.AxisListType

ENGS = ("pe", "act", "pool", "dve", "sp")


class Tile:
    __slots__ = ("name", "h", "w", "r", "dsem", "dcnt", "dlast")

    def __init__(self, name, h):
        self.name = name
        self.h = h
        self.w = []
        self.r = []
        self.dsem = None
        self.dcnt = 0
        self.dlast = None

    def __getitem__(self, idx):
        return self.h[idx]

    def ap(self):
        return self.h.ap() if hasattr(self.h, "ap") else self.h[:]


class Prog:
    def __init__(self, nc, es):
        self.nc = nc
        self.es = es
        self.ops = {e: [] for e in ENGS}
        self.known = {e: {} for e in ENGS}
        self.tiles = {}
        self.dsems = {}
        self.nsem = 0
        self.scopes = []
        self.free_ev = {}
        self.uid = 0
        self.sem_pool = []
        self.sem_free = []

    def _reg(self, name, h):
        t = Tile(name, h)
        self.tiles[name] = t
        return t

    def push(self):
        self.scopes.append((ExitStack(), []))

    def pop(self):
        st, tl = self.scopes.pop()
        for t in tl:
            evs = list(t.w) + list(t.r)
            if t.dlast is not None:
                evs.append(t.dlast)
            for d in evs:
                key = (d[0], d[1])
                if self.free_ev.get(key, -1) < d[2]:
                    self.free_ev[key] = d[2]
            if t.dsem is not None:
                self.sem_pool[t.dsem][1] = t.dcnt
                self.sem_pool[t.dsem][2] = t.dlast
                self.sem_free.append(t.dsem)
            del self.tiles[t.name]
        st.close()

    def sbuf(self, name, shape, dt):
        self.uid += 1
        name = f"{name}_{self.uid}"
        if self.scopes:
            st, tl = self.scopes[-1]
        else:
            st, tl = self.es, None
        import os
        if os.environ.get("DBG_SBUF"):
            import numpy as _np
            bpe = 2 if dt == BF16 else 4
            print("SBUF", name, shape, int(_np.prod(shape[1:])) * bpe, "depth", len(self.scopes))
        t = self._reg(name, st.enter_context(self.nc.sbuf_tensor(name, list(shape), dt)))
        t.w = [(k[0], k[1], v) for k, v in self.free_ev.items()]
        if tl is not None:
            tl.append(t)
        return t

    def psum(self, name, shape, dt):
        return self._reg(name, self.es.enter_context(self.nc.psum_tensor(name, list(shape), dt)))

    def dram(self, name, shape, dt, kind="Internal"):
        return self._reg(name, self.nc.dram_tensor(name, list(shape), dt, kind=kind))

    def _sem(self, name):
        self.nsem += 1
        return self.es.enter_context(self.nc.semaphore(name))

    def _tiles_of(self, aps):
        out = []
        for a in aps:
            if a is None or isinstance(a, (int, float)):
                continue
            if isinstance(a, Tile):
                out.append(a)
                continue
            t = self.tiles.get(a.name)
            if t is not None:
                out.append(t)
        return out

    def _record(self, eng, fn, reads, writes, dma_owner=None):
        rt = self._tiles_of(reads)
        wt = self._tiles_of(writes)
        deps = []
        for t in rt:
            deps.extend(t.w)
        for t in wt:
            deps.extend(t.w)
            deps.extend(t.r)
        if dma_owner is not None:
            if dma_owner.dsem is None:
                if self.sem_free:
                    k = self.sem_free.pop()
                    dma_owner.dsem = k
                    dma_owner.dcnt = self.sem_pool[k][1]
                    dma_owner.dlast = self.sem_pool[k][2]
                else:
                    self.sem_pool.append([self._sem(f"dq{len(self.sem_pool)}"), 0, None])
                    dma_owner.dsem = len(self.sem_pool) - 1
            if dma_owner.dlast is not None:
                deps.append(dma_owner.dlast)
        kn = self.known[eng]
        waits = {}
        for d in deps:
            if d[0] == "op":
                if d[1] == eng and eng == "pe":
                    continue
                key = ("op", d[1])
                v = d[2]
            else:
                key = ("dma", d[1])
                v = d[2]
            if kn.get(key, -1) >= v:
                continue
            if waits.get(key, -1) < v:
                waits[key] = v
        for key, v in waits.items():
            kn[key] = v
            if key[0] == "op":
                self.ops[key[1]][v]["inc"] = True
        idx = len(self.ops[eng])
        rec = {"fn": fn, "waits": list(waits.items()), "inc": False, "dma": None}
        if dma_owner is not None:
            dma_owner.dcnt += 16
            ev = ("dma", dma_owner.dsem, dma_owner.dcnt)
            dma_owner.dlast = ev
            rec["dma"] = self.sem_pool[dma_owner.dsem][0]
        else:
            ev = ("op", eng, idx)
        self.ops[eng].append(rec)
        for t in rt:
            if t not in wt:
                t.r.append(ev)
        for t in wt:
            t.w = [ev]
            t.r = []
        return ev

    def op(self, eng, fn, reads=(), writes=()):
        return self._record(eng, fn, reads, writes)

    def dma(self, eng, out, in_, owner, extra_reads=(), **kw):
        ot = self._tiles_of([owner])[0]
        return self._record(eng, lambda e: e.dma_start(out=out, in_=in_, **kw),
                            [in_] + list(extra_reads), [out], dma_owner=ot)

    def mm(self, out, lhsT, rhs, start=True, stop=True, **kw):
        return self.op("pe", lambda e: e.matmul(out, lhsT, rhs, start=start, stop=stop, **kw),
                       [lhsT, rhs] + ([] if start else [out]), [out])

    def transpose(self, out, in_, ident):
        return self.op("pe", lambda e: e.transpose(out, in_, ident), [in_, ident], [out])

    def act(self, out, in_, func, eng="act", bias=None, scale=None, accum_out=None):
        kw = {}
        if bias is not None:
            kw["bias"] = bias
        if scale is not None:
            kw["scale"] = scale
        if accum_out is not None:
            kw["accum_out"] = accum_out
        return self.op(eng, lambda e: e.activation(out=out, in_=in_, func=func, **kw),
                       [in_, bias, scale], [out, accum_out])

    def tt(self, eng, out, in0, in1, op):
        return self.op(eng, lambda e: e.tensor_tensor(out=out, in0=in0, in1=in1, op=op), [in0, in1], [out])

    def ts(self, eng, out, in0, s1, op0, s2=None, op1=None, accum_out=None):
        def fn(e):
            kw = {}
            if op1 is not None:
                kw["op1"] = op1
            if accum_out is not None:
                kw["accum_out"] = accum_out
            return e.tensor_scalar(out=out, in0=in0, scalar1=s1, scalar2=s2, op0=op0, **kw)
        return self.op(eng, fn, [in0, s1, s2], [out, accum_out])

    def stt(self, eng, out, in0, scalar, in1, op0, op1):
        return self.op(eng, lambda e: e.scalar_tensor_tensor(out=out, in0=in0, scalar=scalar, in1=in1,
                                                             op0=op0, op1=op1), [in0, scalar, in1], [out])

    def copy(self, eng, out, in_):
        if eng == "act":
            return self.op(eng, lambda e: e.activation(out=out, in_=in_, func=AF.Copy), [in_], [out])
        return self.op(eng, lambda e: e.tensor_copy(out=out, in_=in_), [in_], [out])

    def memset(self, eng, out, val):
        return self.op(eng, lambda e: e.memset(out, val), [], [out])

    def finish(self):
        reads = []
        for t in self.tiles.values():
            if t.dlast is not None or t.w or t.r:
                reads.append(t)
        evs = []
        for t in reads:
            evs.extend(t.w)
            evs.extend(t.r)
            if t.dlast is not None:
                evs.append(t.dlast)
        for k, v in self.free_ev.items():
            evs.append((k[0], k[1], v))
        kn = self.known["sp"]
        waits = {}
        for d in evs:
            key = ("op", d[1]) if d[0] == "op" else ("dma", d[1])
            v = d[2]
            if kn.get(key, -1) >= v:
                continue
            if waits.get(key, -1) < v:
                waits[key] = v
        for key, v in waits.items():
            kn[key] = v
            if key[0] == "op":
                self.ops[key[1]][v]["inc"] = True
        self.ops["sp"].append({"fn": None, "waits": list(waits.items()), "inc": False, "dma": None})

    def emit(self):
        nc = self.nc
        esem = {e: self._sem("e_" + e) for e in ENGS}
        vals = {}
        for e in ENGS:
            c = 0
            arr = []
            for o in self.ops[e]:
                if o["inc"]:
                    c += 1
                arr.append(c)
            vals[e] = arr
        block = self.es.enter_context(nc.Block())

        def run(ename, eobj):
            for o in self.ops[ename]:
                for key, v in o["waits"]:
                    if key[0] == "op":
                        eobj.wait_ge(esem[key[1]], vals[key[1]][v])
                    else:
                        eobj.wait_ge(self.sem_pool[key[1]][0], v)
                if o["fn"] is None:
                    continue
                ins = o["fn"](eobj)
                if o["dma"] is not None:
                    ins.then_inc(o["dma"], 16)
                elif o["inc"]:
                    ins.then_inc(esem[ename], 1)

        @block.tensor
        def _(e):
            run("pe", e)

        @block.scalar
        def _(e):
            run("act", e)

        @block.gpsimd
        def _(e):
            run("pool", e)

        @block.vector
        def _(e):
            run("dve", e)

        @block.sync
        def _(e):
            run("sp", e)
        return {e: len(self.ops[e]) for e in ENGS}

import numpy as np

TWO_PI = float(2 * np.pi)


def ssm_stage(P, nc, L):
    din, dout = L["din"], L["dout"]
    next_psf, next_psb = L["next_psf"], L["next_psb"]
    norm_tile, mk_norm_bufs = L["norm_tile"], L["mk_norm_bufs"]
    hT, ident = L["hT"], L["ident"]
    A1_p, sh1_p = L["A1_p"], L["sh1_p"]
    w_in = L["w_in"]
    D, KC, TP, TS, TO = 2048, 16, 1024, 64, 1088

    x_all = din("x_all", [8192, D])
    are_row = din("are_row", [1, 4096])
    aim_row = din("aim_row", [1, 4096])
    ldt_row = din("ldt_row", [1, 4096])
    are_T2 = din("are_T2", [128, 64])
    aim_T2 = din("aim_T2", [128, 64])
    ldt_bc = din("ldt_bc", [128, 64])
    b_T = din("b_T", [16, 2, 4096])
    c_T = din("c_T", [128, 64, 16])
    c_T2 = din("c_T2", [128, 64, 16])
    d_T = din("d_T", [128, 8])
    tau_d = din("tau", [128, 4])
    tri_d = din("tri", [128, 128])
    tris_d = din("tris", [64, 64])
    sels_d = din("sels", [16, 64])
    swap_d = din("swapm", [128, 128])
    oh_d = din("onehot", [128, 64, 8])
    s0_d = din("s0", [64, 8192])
    ssm_p_o = dout("ssm_p", [64, 128])
    ssm_s_o = dout("ssm_s", [64, 8192])
    Bsc = P.dram("Bsc", [16, 2, 4096], BF16)

    P.push()
    tau = P.sbuf("tau", [128, 4], F32)
    P.dma("sp", tau[:, :], tau_d[:, :], tau)
    tri = P.sbuf("tri", [128, 128], BF16)
    P.dma("pool", tri[:, :], tri_d[:, :], tri)
    swapm = P.sbuf("swapm", [128, 128], F32)
    P.dma("sp", swapm[:, :], swap_d[:, :], swapm)
    ones_c = P.sbuf("ones_c", [128, 2], BF16)
    P.memset("dve", ones_c[:, :], 1.0)
    d_sb = P.sbuf("d_sb", [128, 8], F32)
    P.dma("sp", d_sb[:, :], d_T[:, :], d_sb)
    R_sel = P.sbuf("R_sel", [128, 8, 64], F32)
    P.memset("dve", R_sel[:, :, :], 0.0)
    Rsw_sel = P.sbuf("Rsw_sel", [128, 8, 64], F32)
    r_st = P.sbuf("r_st", [128, 64], F32)
    P.memset("dve", r_st[:, :], 0.0)

    def gen_E(R, tcol, g0, ng, E1m=None, E2m=None, E3=None, E4=None, dt_out=BF16):
        P.push()
        tp = [P.sbuf(f"ge{i}", [128, 512], F32) for i in range(8)]
        ti = P.sbuf("gei", [128, 512], I32)
        for pc in range(ng // 8):
            gg = g0 + pc * 8
            cs_ = slice(gg * 64, gg * 64 + 512)
            are_t, aim_t, ldt_t, al, u, sn, cs, tmp = tp
            P.dma("sp", are_t[0:R, :], are_row[0:1, cs_].to_broadcast([R, 512]), are_t)
            P.dma("sp", aim_t[0:R, :], aim_row[0:1, cs_].to_broadcast([R, 512]), aim_t)
            P.dma("sp", ldt_t[0:R, :], ldt_row[0:1, cs_].to_broadcast([R, 512]), ldt_t)
            P.act(ldt_t[0:R, :], ldt_t[0:R, :], AF.Exp)
            P.tt("dve", al[0:R, :], are_t[0:R, :], ldt_t[0:R, :], ALU.mult)
            P.tt("dve", u[0:R, :], aim_t[0:R, :], ldt_t[0:R, :], ALU.mult)
            P.ts("dve", u[0:R, :], u[0:R, :], tau[0:R, tcol:tcol + 1], ALU.mult, 1.0 / TWO_PI, ALU.mult)
            P.copy("dve", ti[0:R, :], u[0:R, :])
            P.copy("dve", tmp[0:R, :], ti[0:R, :])
            P.tt("dve", tmp[0:R, :], u[0:R, :], tmp[0:R, :], ALU.subtract)
            P.act(sn[0:R, :], tmp[0:R, :], AF.Sin, scale=TWO_PI)
            P.ts("dve", u[0:R, :], u[0:R, :], 0.25, ALU.add)
            P.copy("dve", ti[0:R, :], u[0:R, :])
            P.copy("dve", tmp[0:R, :], ti[0:R, :])
            P.tt("dve", tmp[0:R, :], u[0:R, :], tmp[0:R, :], ALU.subtract)
            P.act(cs[0:R, :], tmp[0:R, :], AF.Sin, scale=TWO_PI)
            v3 = lambda t: t[0:R, :].rearrange("p (g q) -> p g q", q=64)
            lo = pc * 8
            if E1m is not None:
                P.act(tmp[0:R, :], al[0:R, :], AF.Exp, scale=tau[0:R, tcol + 1:tcol + 2])
                P.tt("dve", u[0:R, :], tmp[0:R, :], cs[0:R, :], ALU.mult)
                P.copy("act", E1m[0:R, lo:lo + 8, 0, :], v3(u))
                P.copy("pool", E1m[0:R, lo:lo + 8, 1, :], v3(u))
                P.tt("dve", u[0:R, :], tmp[0:R, :], sn[0:R, :], ALU.mult)
                P.copy("pool", E2m[0:R, lo:lo + 8, 0, :], v3(u))
                P.act(E2m[0:R, lo:lo + 8, 1, :], v3(u), AF.Copy, scale=-1.0)
            if E3 is not None:
                P.act(tmp[0:R, :], al[0:R, :], AF.Exp, scale=tau[0:R, tcol:tcol + 1])
                P.tt("dve", u[0:R, :], tmp[0:R, :], cs[0:R, :], ALU.mult)
                P.copy("act", E3[0:R, lo:lo + 8, 0, :], v3(u))
                P.copy("pool", E3[0:R, lo:lo + 8, 1, :], v3(u))
                P.tt("dve", u[0:R, :], tmp[0:R, :], sn[0:R, :], ALU.mult)
                P.copy("pool", E4[0:R, lo:lo + 8, 1, :], v3(u))
                P.act(E4[0:R, lo:lo + 8, 0, :], v3(u), AF.Copy, scale=-1.0)
        P.pop()

    P.push()
    bt = [P.sbuf(f"bb{i}", [16, 1024], F32) for i in range(9)]
    bti = P.sbuf("bbi", [16, 1024], I32)
    Bo = P.sbuf("Bo", [16, 2, 1024], BF16)
    are_t, aim_t, dt_t, abr, abi, t1, t2, zr, zi = bt
    for pcb in range(4):
        csb = slice(pcb * 1024, (pcb + 1) * 1024)
        P.dma("sp", are_t[:, :], are_row[0:1, csb].to_broadcast([16, 1024]), are_t)
        P.dma("sp", aim_t[:, :], aim_row[0:1, csb].to_broadcast([16, 1024]), aim_t)
        P.dma("sp", dt_t[:, :], ldt_row[0:1, csb].to_broadcast([16, 1024]), dt_t)
        P.act(dt_t[:, :], dt_t[:, :], AF.Exp)
        P.tt("dve", t1[:, :], aim_t[:, :], dt_t[:, :], ALU.mult)
        P.ts("dve", t1[:, :], t1[:, :], 1.0 / TWO_PI, ALU.mult)
        P.copy("dve", bti[:, :], t1[:, :])
        P.copy("dve", t2[:, :], bti[:, :])
        P.tt("dve", t2[:, :], t1[:, :], t2[:, :], ALU.subtract)
        P.act(abi[:, :], t2[:, :], AF.Sin, scale=TWO_PI)
        P.ts("dve", t1[:, :], t1[:, :], 0.25, ALU.add)
        P.copy("dve", bti[:, :], t1[:, :])
        P.copy("dve", t2[:, :], bti[:, :])
        P.tt("dve", t2[:, :], t1[:, :], t2[:, :], ALU.subtract)
        P.act(abr[:, :], t2[:, :], AF.Sin, scale=TWO_PI)
        P.tt("dve", t1[:, :], are_t[:, :], dt_t[:, :], ALU.mult)
        P.act(t1[:, :], t1[:, :], AF.Exp)
        P.tt("dve", abr[:, :], abr[:, :], t1[:, :], ALU.mult)
        P.tt("dve", abi[:, :], abi[:, :], t1[:, :], ALU.mult)
        P.ts("dve", abr[:, :], abr[:, :], -1.0, ALU.add)
        P.tt("dve", t1[:, :], are_t[:, :], are_t[:, :], ALU.mult)
        P.tt("dve", t2[:, :], aim_t[:, :], aim_t[:, :], ALU.mult)
        P.tt("dve", t1[:, :], t1[:, :], t2[:, :], ALU.add)
        P.op("dve", lambda e, o=t1[:, :]: e.reciprocal(out=o, in_=o), [t1], [t1])
        P.tt("dve", zr[:, :], abr[:, :], are_t[:, :], ALU.mult)
        P.tt("dve", t2[:, :], abi[:, :], aim_t[:, :], ALU.mult)
        P.tt("dve", zr[:, :], zr[:, :], t2[:, :], ALU.add)
        P.tt("dve", zr[:, :], zr[:, :], t1[:, :], ALU.mult)
        P.tt("dve", zi[:, :], abi[:, :], are_t[:, :], ALU.mult)
        P.tt("dve", t2[:, :], abr[:, :], aim_t[:, :], ALU.mult)
        P.tt("dve", zi[:, :], zi[:, :], t2[:, :], ALU.subtract)
        P.tt("dve", zi[:, :], zi[:, :], t1[:, :], ALU.mult)
        bre, bim = are_t, aim_t
        P.dma("sp", bre[:, :], b_T[:, 0, csb], bre)
        P.dma("sp", bim[:, :], b_T[:, 1, csb], bim)
        P.tt("dve", t1[:, :], zr[:, :], bre[:, :], ALU.mult)
        P.tt("dve", t2[:, :], zi[:, :], bim[:, :], ALU.mult)
        P.tt("dve", Bo[:, 0, :], t1[:, :], t2[:, :], ALU.subtract)
        P.tt("dve", t1[:, :], zr[:, :], bim[:, :], ALU.mult)
        P.tt("dve", t2[:, :], zi[:, :], bre[:, :], ALU.mult)
        P.tt("dve", Bo[:, 1, :], t1[:, :], t2[:, :], ALU.add)
        P.dma("sp", Bsc[:, :, csb], Bo[:, :, :], Bo)
    P.pop()

    def load_BbarBD(dst, g0, ng):
        P.memset("pool", dst[:, :, :, :, :], 0.0)
        Bv = Bsc[:, :, :].rearrange("n c (j l p) -> n c j l p", l=8, p=64)
        for gl in range(8):
            for c in range(2):
                P.dma("sp", dst[16 * gl:16 * gl + 16, :, gl, c, :], Bv[:, c, g0 // 8:(g0 + ng) // 8, gl, :], dst,
                      extra_reads=[Bsc])

    Ar = P.sbuf("Ar", [128, 64], F32)
    Ai = P.sbuf("Ai", [128, 64], F32)
    P.push()
    ct = [P.sbuf(f"ct{i}", [128, 64], F32) for i in range(6)]
    cti = P.sbuf("cti", [128, 64], I32)
    a_t, b_t, dtt, w1, w2, w3 = ct
    P.dma("sp", a_t[:, :], are_T2[:, :], a_t)
    P.dma("sp", b_t[:, :], aim_T2[:, :], b_t)
    P.dma("sp", dtt[:, :], ldt_bc[:, :], dtt)
    P.act(dtt[:, :], dtt[:, :], AF.Exp)
    P.tt("dve", a_t[:, :], a_t[:, :], dtt[:, :], ALU.mult)
    P.tt("dve", b_t[:, :], b_t[:, :], dtt[:, :], ALU.mult)
    P.ts("dve", b_t[:, :], b_t[:, :], 128.0 / TWO_PI, ALU.mult)
    P.copy("dve", cti[:, :], b_t[:, :])
    P.copy("dve", w1[:, :], cti[:, :])
    P.tt("dve", w1[:, :], b_t[:, :], w1[:, :], ALU.subtract)
    P.act(w2[:, :], w1[:, :], AF.Sin, scale=TWO_PI)
    P.ts("dve", b_t[:, :], b_t[:, :], 0.25, ALU.add)
    P.copy("dve", cti[:, :], b_t[:, :])
    P.copy("dve", w1[:, :], cti[:, :])
    P.tt("dve", w1[:, :], b_t[:, :], w1[:, :], ALU.subtract)
    P.act(w3[:, :], w1[:, :], AF.Sin, scale=TWO_PI)
    P.act(w1[:, :], a_t[:, :], AF.Exp, scale=128.0)
    P.tt("dve", Ar[:, :], w1[:, :], w3[:, :], ALU.mult)
    P.tt("dve", Ai[:, :], w1[:, :], w2[:, :], ALU.mult)
    P.ts("dve", Ai[0:64, :], Ai[0:64, :], -1.0, ALU.mult)
    P.pop()

    P.push()
    E1m = P.sbuf("E1m", [128, 64, 2, 64], BF16)
    E2m = P.sbuf("E2m", [128, 64, 2, 64], BF16)
    gen_E(128, 0, 0, 64, E1m=E1m, E2m=E2m)
    BBD = P.sbuf("BBD", [128, 8, 8, 2, 64], BF16)
    load_BbarBD(BBD, 0, 64)
    Wu = P.sbuf("Wu", [128, KC, 1024], BF16)
    P.dma("pool", Wu[:, :, 0:512], w_in[:, 0:512].rearrange("(k p) n -> p k n", p=128), Wu)
    P.dma("pool", Wu[:, :, 512:1024], w_in[:, 512:1024].rearrange("(k p) n -> p k n", p=128), Wu)
    mk_norm_bufs(1)
    hTg = [P.sbuf(f"hTg{i}", [128, KC, 128], BF16) for i in range(1)] * 2
    uTg = [P.sbuf(f"uTg{i}", [128, 8, 128], BF16) for i in range(2)]
    W1 = P.sbuf("W1", [128, 32, 2, 64], BF16)
    W2 = P.sbuf("W2", [128, 32, 2, 64], BF16)
    W3 = P.sbuf("W3", [128, 32, 2, 64], BF16)
    tch = P.sbuf("tch", [128, 64], F32)
    tch2 = P.sbuf("tch2", [128, 64], F32)
    tsel = P.sbuf("tsel", [128, 8, 64], F32)
    oh = P.sbuf("oh", [128, 64, 8], F32)
    P.dma("sp", oh[:, :, :], oh_d[:, :, :], oh)

    def chunk_X(u_src, h, Ea, Eb, BB, R=128):
        for jj in range(4):
            j = h * 4 + jj
            for half in range(2):
                ps = next_psf()
                P.mm(ps[0:R, :], u_src(j), BB[:, jj if BB is not BBD else j, half * 4:(half + 1) * 4, :, :], start=True, stop=True)
                dst = W1[0:R, jj * 8 + half * 4:jj * 8 + half * 4 + 4, :, :]
                if (jj + half) % 2 == 0:
                    P.copy("act", dst, ps[0:R, :].rearrange("p (g c q) -> p g c q", c=2, q=64))
                else:
                    P.copy("dve", dst, ps[0:R, :].rearrange("p (g c q) -> p g c q", c=2, q=64))
        P.tt("dve", W2[0:R, :, :, :], Ea[0:R, :, :, :], W1[0:R, :, :, :], ALU.mult)
        P.tt("dve", W3[0:R, :, 0, :], Eb[0:R, :, 0, :], W1[0:R, :, 1, :], ALU.mult)
        P.tt("dve", W3[0:R, :, 1, :], Eb[0:R, :, 1, :], W1[0:R, :, 0, :], ALU.mult)

    import os
    NCK = int(os.environ.get("DBG_CHUNKS", "64"))
    dbg = NCK < 64
    if dbg:
        d_u = dout("d_u", [128, 8, 128], BF16)
        d_w1 = dout("d_w1", [128, 8192], BF16)
        d_w2 = dout("d_w2", [128, 8192], BF16)
        d_w3 = dout("d_w3", [128, 8192], BF16)
        d_c = dout("d_c", [128, 64])
        d_r = dout("d_r", [128, 2, 64])
        d_e1 = dout("d_e1", [128, 8192], BF16)
        d_e2 = dout("d_e2", [128, 8192], BF16)
        d_bbd = dout("d_bbd", [128, 8192], BF16)
        P.dma("sp", d_e1[:, :], E1m[:, :, :, :].rearrange("p g c q -> p (g c q)"), E1m)
        P.dma("sp", d_e2[:, :], E2m[:, :, :, :].rearrange("p g c q -> p (g c q)"), E2m)
        P.dma("sp", d_bbd[:, :], BBD[:, :, :, :, :].rearrange("p j l c q -> p (j l c q)"), BBD)
    for ck in range(NCK):
        hh = hTg[ck % 2]
        uu = uTg[ck % 2]
        norm_tile(x_all[ck * 128:(ck + 1) * 128, :], 128, A1_p, sh1_p, lambda kc0, hh=hh: hh[:, kc0:kc0 + 8, :])
        for jq in range(2):
            ps = next_psf()
            for jj in range(4):
                j = jq * 4 + jj
                for kc in range(KC):
                    P.mm(ps[:, jj * 128:(jj + 1) * 128], Wu[:, kc, j * 128:(j + 1) * 128], hh[:, kc, :],
                         start=(kc == 0), stop=(kc == KC - 1))
            P.copy("act", uu[:, jq * 4:jq * 4 + 4, :], ps[:, :].rearrange("p (j n) -> p j n", n=128))
        psc = L['psx']
        for h in range(2):
            chunk_X(lambda j, uu=uu: uu[:, j, :], h, E1m[:, h * 32:(h + 1) * 32], E2m[:, h * 32:(h + 1) * 32], BBD)
            for g in range(32):
                gg = h * 32 + g
                P.mm(psc[:, gg:gg + 1], W2[:, g, :, :], ones_c[:, 0:1], start=True, stop=False)
                P.mm(psc[:, gg:gg + 1], W3[:, g, :, :], ones_c[:, 0:1], start=False, stop=True)
            if dbg and ck == 0:
                P.dma("sp", d_w1[:, h * 4096:(h + 1) * 4096], W1[:, :, :, :].rearrange("p g c q -> p (g c q)"), W1)
                P.dma("sp", d_w2[:, h * 4096:(h + 1) * 4096], W2[:, :, :, :].rearrange("p g c q -> p (g c q)"), W2)
                P.dma("sp", d_w3[:, h * 4096:(h + 1) * 4096], W3[:, :, :, :].rearrange("p g c q -> p (g c q)"), W3)
        if dbg and ck == 0:
            P.dma("sp", d_u[:, :, :], uu[:, :, :], uu)
            dcs = P.sbuf("dcs", [128, 64], F32)
            P.copy("dve", dcs[:, :], psc[:, 0:64])
            P.dma("sp", d_c[:, :], dcs[:, :], dcs)
        P.tt("pool", tsel[:, :, :], r_st[:, :].unsqueeze(1).to_broadcast([128, 8, 64]),
             oh[:, ck, :].unsqueeze(2).to_broadcast([128, 8, 64]), ALU.mult)
        P.tt("pool", R_sel[:, :, :], R_sel[:, :, :], tsel[:, :, :], ALU.add)
        P.tt("dve", tch[:, :], r_st[:, :], psc[:, 0:64], ALU.add)
        pss = next_psf()
        P.mm(pss[:, 0:64], swapm[:, :], tch[:, :], start=True, stop=True)
        P.tt("dve", tch2[:, :], Ai[:, :], pss[:, 0:64], ALU.mult)
        P.tt("dve", tch[:, :], Ar[:, :], tch[:, :], ALU.mult)
        P.tt("dve", r_st[:, :], tch[:, :], tch2[:, :], ALU.add)
        if dbg and ck < 2:
            P.dma("sp", d_r[:, ck, :], r_st[:, :], r_st)
    psf_ = next_psf()
    identf = P.sbuf("identf", [128, 128], F32)
    P.dma("sp", identf[:, :], L["ident_d"][:, :], identf)
    P.op("pe", lambda e: e.transpose(psf_[0:64, 0:128], r_st[:, :], identf[:, :]), [r_st, identf], [psf_])
    fin = P.sbuf("fin", [64, 128], F32)
    P.copy("dve", fin[:, :], psf_[0:64, 0:128])
    P.dma("sp", ssm_p_o[:, :], fin[:, :], fin)
    for i in range(8):
        pss = next_psf()
        P.mm(pss[:, 0:64], swapm[:, :], R_sel[:, i, :], start=True, stop=True)
        P.copy("dve", Rsw_sel[:, i, :], pss[:, 0:64])
    P.pop()

    ygT = P.sbuf("ygT", [128, 8, TO], BF16)
    L["ygT"] = ygT
    L["G"]["ygT"] = ygT
    if L["stage"] >= 3:
        ssm_own(P, nc, L, locals())


def ssm_own(P, nc, L, S):
    next_psf, next_psb = L["next_psf"], L["next_psb"]
    hT, ident, ygT, w_in = L["hT"], L["ident"], L["ygT"], L["w_in"]
    gen_E, load_BbarBD = S["gen_E"], S["load_BbarBD"]
    tau, tri, ones_c, d_sb, R_sel, Rsw_sel = S["tau"], S["tri"], S["ones_c"], S["d_sb"], S["R_sel"], S["Rsw_sel"]
    are_row, aim_row, ldt_row = S["are_row"], S["aim_row"], S["ldt_row"]
    s0_d, ssm_s_o, c_T, c_T2, tris_d = S["s0_d"], S["ssm_s_o"], S["c_T"], S["c_T2"], S["tris_d"]
    KC, TO = 16, 1088
    NG = 16

    P.push()
    uT = P.sbuf("uT", [128, 8, TO], BF16)
    P.push()
    Wu = P.sbuf("Wu2", [128, KC, 1024], BF16)
    P.dma("pool", Wu[:, :, 0:512], w_in[:, 0:512].rearrange("(k p) n -> p k n", p=128), Wu)
    P.dma("pool", Wu[:, :, 512:1024], w_in[:, 512:1024].rearrange("(k p) n -> p k n", p=128), Wu)
    for j in range(8):
        for (t0, n) in ((0, 512), (512, 512), (1024, 64)):
            ps = next_psf()
            for kc in range(KC):
                P.mm(ps[:, 0:n], Wu[:, kc, j * 128:(j + 1) * 128], hT[:, kc, t0:t0 + n], start=(kc == 0), stop=(kc == KC - 1))
            P.copy("act", uT[:, j, t0:t0 + n], ps[:, 0:n])
    P.pop()

    tris = P.sbuf("tris", [64, 64], BF16)
    P.dma("pool", tris[:, :], tris_d[:, :], tris)
    CT = P.sbuf("CT", [128, 64, 16], F32)
    CT2 = P.sbuf("CT2", [128, 64, 16], F32)
    P.dma("sp", CT[:, :, :], c_T[:, :, :], CT)
    P.dma("sp", CT2[:, :, :], c_T2[:, :, :], CT2)
    P.ts("dve", CT[64:128, :, :], CT[64:128, :, :], -1.0, ALU.mult)
    P.ts("dve", CT2[:, :, :], CT2[:, :, :], -1.0, ALU.mult)
    Aall = P.sbuf("Aall", [128, 8, 64], F32)
    Ball = P.sbuf("Ball", [128, 8, 64], F32)
    P.copy("dve", Aall[0:64, :, :], R_sel[0:64, :, :])
    P.copy("dve", Aall[64:128, :, :], Rsw_sel[64:128, :, :])
    P.copy("dve", Ball[0:64, :, :], Rsw_sel[0:64, :, :])
    P.copy("dve", Ball[64:128, :, :], R_sel[64:128, :, :])

    E1m = P.sbuf("qE1m", [128, NG, 2, 64], BF16)
    E2m = P.sbuf("qE2m", [128, NG, 2, 64], BF16)
    E3 = P.sbuf("qE3", [128, NG, 2, 64], BF16)
    E4 = P.sbuf("qE4", [128, NG, 2, 64], BF16)
    BBq = P.sbuf("BBq", [128, 2, 8, 2, 64], BF16)
    Cpad = P.sbuf("Cpad", [128, NG, 128], BF16)
    CSp = P.sbuf("CSp", [128, NG, 128], BF16)
    EpT = P.sbuf("EpT", [128, NG, 128], BF16)
    W1 = P.sbuf("qW1", [128, NG, 2, 64], BF16)
    W2 = P.sbuf("qW2", [128, NG, 2, 64], BF16)
    W3 = P.sbuf("qW3", [128, NG, 2, 64], BF16)
    sb = P.sbuf("qsb", [128, NG, 2, 64], BF16)
    Ecat = W3[:, :, :, :].rearrange("p g c q -> p g (c q)")
    sT = P.sbuf("qsT", [128, NG, 128], BF16)
    csf = P.sbuf("csf", [128, NG, 16], F32)
    csf2 = P.sbuf("csf2", [128, NG, 16], F32)
    y32 = P.sbuf("y32", [128, 2, 128], F32)
    g32 = P.sbuf("g32", [128, 2, 128], F32)
    T0 = P.sbuf("T0", [64, NG, 2, 64], F32)
    s32 = T0

    def chunk_X(u_src, R):
        for jj in range(NG // 8):
            for half in range(2):
                ps = next_psf()
                P.mm(ps[0:R, :], u_src(jj), BBq[:, jj, half * 4:(half + 1) * 4, :, :], start=True, stop=True)
                dst = W1[0:R, jj * 8 + half * 4:jj * 8 + half * 4 + 4, :, :]
                P.copy("act" if (jj + half) % 2 == 0 else "dve", dst,
                       ps[0:R, :].rearrange("p (g c q) -> p g c q", c=2, q=64))
        P.tt("dve", W2[0:R, :, :, :], E1m[0:R, :, :, :], W1[0:R, :, :, :], ALU.mult)
        P.tt("dve", W3[0:R, :, 0, :], E2m[0:R, :, 0, :], W1[0:R, :, 1, :], ALU.mult)
        P.tt("dve", W3[0:R, :, 1, :], E2m[0:R, :, 1, :], W1[0:R, :, 0, :], ALU.mult)

    def cumsum_Z(R, trimat, extra=None):
        W2f = W2[0:R, :, :, :].rearrange("p g c q -> p (g c q)")
        W3f = W3[0:R, :, :, :].rearrange("p g c q -> p (g c q)")
        W1f = W1[0:R, :, :, :].rearrange("p g c q -> p (g c q)")
        for blk in range(NG * 128 // 512):
            ps = next_psf()
            cs_ = slice(blk * 512, (blk + 1) * 512)
            P.mm(ps[0:R, :], trimat, W2f[:, cs_], start=True, stop=False)
            P.mm(ps[0:R, :], trimat, W3f[:, cs_], start=False, stop=True)
            P.copy("act" if blk % 2 == 0 else "dve", W1f[:, cs_], ps[0:R, :])

    def apply_Ep(R, out_t):
        P.tt("dve", W2[0:R, :, :, :], E3[0:R, :, :, :], W1[0:R, :, :, :], ALU.mult)
        P.tt("dve", W3[0:R, :, 0, :], E4[0:R, :, 0, :], W1[0:R, :, 1, :], ALU.mult)
        P.tt("dve", W3[0:R, :, 1, :], E4[0:R, :, 1, :], W1[0:R, :, 0, :], ALU.mult)
        P.tt("dve", out_t[0:R, :, :, :], W2[0:R, :, :, :], W3[0:R, :, :, :], ALU.add)

    def transpose_s(R):
        for half in range(NG // 8):
            pt = next_psb()
            for k in range(8):
                g = half * 8 + k
                P.transpose(pt[:, k * 128:k * 128 + R], sb[0:R, g, :, :].rearrange("p c q -> p (c q)"), ident[0:R, 0:R])
            P.copy("act", sT[:, half * 8:half * 8 + 8, 0:R], pt[:, :].rearrange("p (k n) -> p k n", n=128)[:, :, 0:R])

    def y_out(qd, t0, R, with_carry):
        for jq in range(NG // 8):
            j = qd * (NG // 8) + jq
            ps = next_psf()
            n_mm = 16 if with_carry else 8
            k = 0
            for gl in range(8):
                P.mm(ps[:, 0:R], Cpad[:, jq * 8 + gl, :], sT[:, jq * 8 + gl, 0:R], start=(k == 0), stop=(k == n_mm - 1))
                k += 1
            if with_carry:
                for gl in range(8):
                    P.mm(ps[:, 0:R], CSp[:, jq * 8 + gl, :], EpT[:, jq * 8 + gl, 0:R], start=False, stop=(k == n_mm - 1))
                    k += 1
            P.stt("dve", y32[:, jq, 0:R], uT[:, j, t0:t0 + R], d_sb[:, j:j + 1], ps[:, 0:R], ALU.mult, ALU.add)
        P.tt("dve", g32[:, :, 0:R], y32[:, :, 0:R], y32[:, :, 0:R], ALU.mult)
        P.ts("dve", g32[:, :, 0:R], g32[:, :, 0:R], 0.044715, ALU.mult, 1.0, ALU.add)
        P.tt("dve", g32[:, :, 0:R], g32[:, :, 0:R], y32[:, :, 0:R], ALU.mult)
        P.act(g32[:, :, 0:R], g32[:, :, 0:R], AF.Sigmoid, scale=1.5957691216)
        j0 = qd * (NG // 8)
        P.tt("dve", ygT[:, j0:j0 + NG // 8, t0:t0 + R], g32[:, :, 0:R], y32[:, :, 0:R], ALU.mult)

    for qd in range(64 // NG):
        g0 = qd * NG
        gen_E(128, 0, g0, NG, E1m=E1m, E2m=E2m, E3=E3, E4=E4)
        load_BbarBD(BBq, g0, NG)
        P.memset("pool", Cpad[:, :, :], 0.0)
        P.memset("pool", CSp[:, :, :], 0.0)
        for g in range(NG):
            gl = g % 8
            P.copy("pool", Cpad[:, g, 16 * gl:16 * gl + 16], CT[:, g0 + g, :])
        P.copy("dve", W3[:, :, 0, :], E3[:, :, 0, :])
        P.copy("dve", W3[:, :, 1, :], E4[:, :, 1, :])
        for half in range(NG // 8):
            pt = next_psb()
            for k in range(8):
                P.transpose(pt[:, k * 128:(k + 1) * 128], Ecat[:, half * 8 + k, :], ident[:, :])
            P.copy("act", EpT[:, half * 8:half * 8 + 8, :], pt[:, :].rearrange("p (k n) -> p k n", n=128))
        for i in range(8):
            chunk_X(lambda jj, i=i, qd=qd: uT[:, qd * (NG // 8) + jj, i * 128:(i + 1) * 128], 128)
            cumsum_Z(128, tri[:, :])
            apply_Ep(128, sb)
            transpose_s(128)
            P.tt("dve", csf[:, :, :], CT[:, g0:g0 + NG, :], Aall[:, i, g0:g0 + NG].unsqueeze(2).to_broadcast([128, NG, 16]), ALU.mult)
            P.tt("dve", csf2[:, :, :], CT2[:, g0:g0 + NG, :], Ball[:, i, g0:g0 + NG].unsqueeze(2).to_broadcast([128, NG, 16]), ALU.mult)
            P.tt("dve", csf[:, :, :], csf[:, :, :], csf2[:, :, :], ALU.add)
            for g in range(NG):
                gl = g % 8
                P.copy("pool", CSp[:, g, 16 * gl:16 * gl + 16], csf[:, g, :])
            y_out(qd, i * 128, 128, True)
        gen_E(64, 2, g0, NG, E1m=E1m, E2m=E2m, E3=E3, E4=E4)
        chunk_X(lambda jj, qd=qd: uT[:, qd * (NG // 8) + jj, 1024:1088], 64)
        cumsum_Z(64, tris[:, :])
        apply_Ep(64, sb)
        P.push()
        tp = [P.sbuf(f"sx{i}", [64, 512], F32) for i in range(8)]
        ti = P.sbuf("sxi", [64, 512], I32)
        s0t = P.sbuf("s0t", [64, 8, 2, 64], F32)
        for pc in range(NG // 8):
            gg = g0 + pc * 8
            cs_ = slice(gg * 64, gg * 64 + 512)
            are_t, aim_t, ldt_t, al, u, sn, cs, tmp = tp
            tmp2 = are_t
            P.dma("sp", are_t[:, :], are_row[0:1, cs_].to_broadcast([64, 512]), are_t)
            P.dma("sp", aim_t[:, :], aim_row[0:1, cs_].to_broadcast([64, 512]), aim_t)
            P.dma("sp", ldt_t[:, :], ldt_row[0:1, cs_].to_broadcast([64, 512]), ldt_t)
            P.dma("sp", s0t[:, :, :, :].rearrange("p g c q -> p (g c q)"), s0_d[:, gg * 128:gg * 128 + 1024], s0t)
            P.act(ldt_t[:, :], ldt_t[:, :], AF.Exp)
            P.tt("dve", al[:, :], are_t[:, :], ldt_t[:, :], ALU.mult)
            P.tt("dve", u[:, :], aim_t[:, :], ldt_t[:, :], ALU.mult)
            P.ts("dve", u[:, :], u[:, :], tau[0:64, 2:3], ALU.mult, 1.0 / TWO_PI, ALU.mult)
            P.copy("dve", ti[:, :], u[:, :])
            P.copy("dve", tmp[:, :], ti[:, :])
            P.tt("dve", tmp[:, :], u[:, :], tmp[:, :], ALU.subtract)
            P.act(sn[:, :], tmp[:, :], AF.Sin, scale=TWO_PI)
            P.ts("dve", u[:, :], u[:, :], 0.25, ALU.add)
            P.copy("dve", ti[:, :], u[:, :])
            P.copy("dve", tmp[:, :], ti[:, :])
            P.tt("dve", tmp[:, :], u[:, :], tmp[:, :], ALU.subtract)
            P.act(cs[:, :], tmp[:, :], AF.Sin, scale=TWO_PI)
            P.act(tmp[:, :], al[:, :], AF.Exp, scale=tau[0:64, 2:3])
            P.tt("dve", cs[:, :], cs[:, :], tmp[:, :], ALU.mult)
            P.tt("dve", sn[:, :], sn[:, :], tmp[:, :], ALU.mult)
            v3 = lambda t: t[:, :].rearrange("p (g q) -> p g q", q=64)
            lo = pc * 8
            P.tt("dve", v3(tmp), v3(cs), s0t[:, :, 0, :], ALU.mult)
            P.tt("dve", v3(tmp2), v3(sn), s0t[:, :, 1, :], ALU.mult)
            P.tt("dve", T0[:, lo:lo + 8, 0, :], v3(tmp), v3(tmp2), ALU.subtract)
            P.tt("dve", v3(tmp), v3(cs), s0t[:, :, 1, :], ALU.mult)
            P.tt("dve", v3(tmp2), v3(sn), s0t[:, :, 0, :], ALU.mult)
            P.tt("dve", T0[:, lo:lo + 8, 1, :], v3(tmp), v3(tmp2), ALU.add)
        P.pop()
        P.tt("dve", T0[:, :, :, :], T0[:, :, :, :], W2[0:64, :, :, :], ALU.add)
        P.tt("dve", T0[:, :, :, :], T0[:, :, :, :], W3[0:64, :, :, :], ALU.add)
        P.dma("sp", ssm_s_o[:, g0 * 128:(g0 + NG) * 128], s32[:, :, :, :].rearrange("p g c q -> p (g c q)"), s32)
        P.copy("dve", sb[0:64, :, :, :], s32[:, :, :, :])
        transpose_s(64)
        y_out(qd, 1024, 64, False)
    if L["stage"] == 3:
        d_yg = L["dout"]("d_yg", [128, 8, TO], BF16)
        P.dma("sp", d_yg[:, :, :], ygT[:, :, :], ygT)
    P.pop()

SQ = float(1.0 / np.sqrt(128.0))


def sl(start, n, step):
    return slice(start, start + (n - 1) * step + 1, step)


def attn_setup(P, nc, L):
    din = L["din"]
    TO = 1088
    a = {}
    a["cache"] = [din("cache0", [16, 128, 1024]), din("cache1", [16, 512, 1024]), din("cache2", [16, 2048, 1024])]
    mA_d = din("maskA", [128, 128])
    mB_d = din("maskB", [128, 128])
    mC_d = din("maskC", [128, 16])
    mN_d = din("maskN", [64, 2, 16, 16])
    vA_d = [din(f"maskAv{g}", [128, (1, 4, 16)[g], 128]) for g in range(3)]
    a["UT"] = P.sbuf("UT_acc", [128, 4, TO], F32)
    a["LT"] = P.sbuf("L_acc", [128, 4, TO], F32)
    a["mA"] = P.sbuf("mA", [128, 128], BF16)
    a["mB"] = P.sbuf("mB", [128, 128], BF16)
    a["mC"] = P.sbuf("mC", [128, 16], BF16)
    a["mN"] = P.sbuf("mN", [64, 2, 16, 16], BF16)
    a["ones"] = P.sbuf("ones128", [128, 128], BF16)
    P.dma("pool", a["mA"][:, :], mA_d[:, :], a["mA"])
    P.dma("pool", a["mB"][:, :], mB_d[:, :], a["mB"])
    P.dma("pool", a["mC"][:, :], mC_d[:, :], a["mC"])
    P.dma("pool", a["mN"][:, :, :, :], mN_d[:, :, :, :], a["mN"])
    P.memset("dve", a["ones"][:, :], 1.0)
    a["vA"] = []
    for g in range(3):
        t = P.sbuf(f"maskAv{g}_sb", [128, (1, 4, 16)[g], 128], BF16)
        P.dma("pool", t[:, :, :], vA_d[g][:, :, :], t)
        a["vA"].append(t)
    negB = P.sbuf("negB", [128, 2], F32)
    qg_bc, kg_bc = L["qg_bc"], L["kg_bc"]
    P.op("dve", lambda e: e.tensor_reduce(out=negB[:, 0:1], in_=qg_bc[:, :], axis=AX.X, op=ALU.max,
                                          apply_absolute_value=True), [qg_bc], [negB])
    P.op("dve", lambda e: e.tensor_reduce(out=negB[:, 1:2], in_=kg_bc[:, :], axis=AX.X, op=ALU.max,
                                          apply_absolute_value=True), [kg_bc], [negB])
    P.tt("dve", negB[:, 0:1], negB[:, 0:1], negB[:, 1:2], ALU.mult)
    P.ts("dve", negB[:, 0:1], negB[:, 0:1], -float(np.sqrt(128.0)), ALU.mult)
    a["negB"] = negB
    return a


def attn_group(P, nc, L, a, g):
    next_psf, next_psb = L["next_psf"], L["next_psb"]
    KT, QT, KTs, QTs, Vs, Vd, ident = L["KT"][g], L["QT"][g], L["KTs"][g], L["QTs"][g], L["Vs"][g], L["Vd"][g], L["ident"]
    UT, LT, mA, mB, mC, mN, ones, negB = a["UT"], a["LT"], a["mA"], a["mB"], a["mC"], a["mN"], a["ones"], a["negB"]
    d = (1, 4, 16)[g]
    nq = 1024 // d
    nblk = max(1, nq // 128)
    bq = min(128, nq)
    first = (g == 0)
    import os as _os
    parts = _os.environ.get("ATT_PARTS", "ps")
    if str(g) not in _os.environ.get("ATT_G", "012"):
        parts = parts.replace("p", "")
    P.push()
    vAt = [P.sbuf(f"vAt{i}", [128, 512], BF16) for i in range(2)]
    vBt = [P.sbuf(f"vBt{i}", [128, 512], BF16) for i in range(2)]
    eA = [P.sbuf(f"eA{i}", [128, 128], BF16) for i in range(2)]
    eB = [P.sbuf(f"eB{i}", [128, 128], BF16) for i in range(2)]
    pA = [P.sbuf(f"pA{i}", [128, 128], BF16) for i in range(2)]
    pB = [P.sbuf(f"pB{i}", [128, 128], BF16) for i in range(2)]
    it = 0
    if d > 1:
        M = 128 + nq
        KTd = P.sbuf("KTd", [128, 4, d, M], BF16)
        QTd = P.sbuf("QTd", [128, 4, d, nq], BF16)
        for j in range(4):
            P.copy("act" if j % 2 == 0 else "dve", KTd[:, j, :, :], KT[:, j, 0:d * M].rearrange("p (m r) -> p r m", r=d))
            P.copy("dve" if j % 2 == 0 else "act", QTd[:, j, :, :], QT[:, j, 0:d * nq].rearrange("p (m r) -> p r m", r=d))
    for r in (range(d) if "p" in parts else []):
        for blk in range(nblk):
            Q0 = 128 + blk * 128
            kkA0 = r + d * (Q0 - 128)
            kkB0 = r + d * Q0
            va, vb_ = vAt[(r * nblk + blk) % 2], vBt[(r * nblk + blk) % 2]
            if _os.environ.get("ATT_NOVD"):
                P.memset("pool", va[:, :], 0.5)
                P.memset("pool", vb_[:, :], 0.5)
            else:
                P.dma("pool", va[:, :], Vd[sl(kkA0, 128, d), :], va)
                P.dma("pool", vb_[0:bq, :], Vd[sl(kkB0, bq, d), :], vb_)
            q0 = r + d * blk * 128
            for j in range(4):
                i = it % 2
                it += 1
                if d > 1:
                    qc = QTd[:, j, r, blk * 128:blk * 128 + bq]
                    kA_ = KTd[:, j, r, Q0 - 128:Q0]
                    kB_ = KTd[:, j, r, Q0:Q0 + bq]
                else:
                    qc = QT[:, j, q0:q0 + bq]
                    kA_ = KT[:, j, kkA0:kkA0 + 128]
                    kB_ = KT[:, j, kkB0:kkB0 + bq]
                ps = next_psf()
                P.mm(ps[:, 0:bq], kA_, qc, start=True, stop=True)
                P.mm(ps[0:bq, 128:128 + bq], kB_, qc, start=True, stop=True)
                P.act(eA[i][:, 0:bq], ps[:, 0:bq], AF.Exp, scale=SQ, bias=negB[:, 0:1])
                P.act(eB[i][0:bq, 0:bq], ps[0:bq, 128:128 + bq], AF.Exp, scale=SQ, bias=negB[0:bq, 0:1])
                if blk == 0:
                    P.tt("dve", pA[i][:, 0:bq], eA[i][:, 0:bq], a["vA"][g][:, r, 0:bq], ALU.mult)
                else:
                    P.tt("dve", pA[i][:, 0:bq], eA[i][:, 0:bq], mA[:, 0:bq], ALU.mult)
                P.tt("pool", pB[i][0:bq, 0:bq], eB[i][0:bq, 0:bq], mB[0:bq, 0:bq], ALU.mult)
                if _os.environ.get("ATT_CUT") == "1":
                    continue
                pu = next_psf()
                P.mm(pu[:, 0:bq], va[:, j * 128:(j + 1) * 128], pA[i][:, 0:bq], start=True, stop=False)
                P.mm(pu[:, 0:bq], vb_[0:bq, j * 128:(j + 1) * 128], pB[i][0:bq, 0:bq], start=False, stop=True)
                P.mm(pu[:, 128:128 + bq], ones[:, :], pA[i][:, 0:bq], start=True, stop=False)
                P.mm(pu[:, 128:128 + bq], ones[0:bq, :], pB[i][0:bq, 0:bq], start=False, stop=True)
                if _os.environ.get("ATT_CUT") == "2":
                    continue
                uo = UT[:, j, sl(q0, bq, d)]
                lo = LT[:, j, sl(q0, bq, d)]
                if first:
                    P.copy("act", uo, pu[:, 0:bq])
                    P.copy("act", lo, pu[:, 128:128 + bq])
                else:
                    P.tt("dve", uo, pu[:, 0:bq], uo, ALU.add)
                    P.tt("dve", lo, pu[:, 128:128 + bq], lo, ALU.add)
    P.pop()

    P.push()
    cache = a["cache"][g]
    if g == 0:
        ct = [P.sbuf(f"ctile{i}", [128, 1, 1024], BF16) for i in range(2)]
    else:
        ct = [P.sbuf(f"ctile{i}", [128, 4, 1024], BF16) for i in range(2)]
    ns = 1 if g == 0 else 4
    KcT = [P.sbuf(f"KcT{i}", [128, ns, 4, 128], BF16) for i in range(2)]
    eC = [P.sbuf(f"eC{i}", [128, 16], BF16) for i in range(2)]
    eN = [P.sbuf(f"eN{i}", [64, 16], BF16) for i in range(2)]
    pN = [P.sbuf(f"pN{i}", [64, 16], BF16) for i in range(2)]
    pC = [P.sbuf(f"pC{i}", [128, 16], BF16) for i in range(2)]
    for n in (range(16) if "s" in parts else []):
        i = n % 2
        c = ct[i]
        if g == 0:
            P.dma("pool", c[:, 0, :], cache[n, :, :], c)
        elif g == 1:
            P.dma("pool", c[:, :, :], cache[n, :, :].rearrange("(m s) f -> m s f", s=4), c)
        else:
            P.dma("pool", c[:, :, :], cache[n, :, :].rearrange("(m t) f -> m t f", t=16)[:, 0:4, :], c)
        for s in range(ns):
            pt = next_psb()
            for j in range(4):
                P.transpose(pt[:, j * 128:(j + 1) * 128], c[:, s, j * 128:(j + 1) * 128], ident[:, :])
            P.copy("act", KcT[i][:, s, :, :], pt[:, 0:512].rearrange("p (j n) -> p j n", n=128))
        ps = next_psf()
        for j in range(4):
            qn = QTs[:, j, 4 * n:4 * n + 4]
            if g == 0:
                P.mm(ps[:, j * 4:(j + 1) * 4], KcT[i][:, 0, j, :], qn, start=True, stop=True)
            else:
                for s in range(4):
                    P.mm(ps[:, j * 4 + s:j * 4 + s + 1], KcT[i][:, s, j, :], QTs[:, j, 4 * n + s:4 * n + s + 1],
                         start=True, stop=True)
            P.mm(ps[0:64, 16 + j * 4:16 + (j + 1) * 4], KTs[:, j, :], qn, start=True, stop=True)
        P.act(eC[i][:, :], ps[:, 0:16], AF.Exp, scale=SQ, bias=negB[:, 0:1])
        P.act(eN[i][:, :], ps[0:64, 16:32], AF.Exp, scale=SQ, bias=negB[0:64, 0:1])
        if g == 0:
            P.tt("dve", pC[i][:, :], eC[i][:, :], mC[:, :], ALU.mult)
            pc_ = pC[i]
        else:
            pc_ = eC[i]
        P.tt("pool", pN[i][:, :], eN[i][:, :], mN[:, 0 if g == 0 else 1, n, :], ALU.mult)
        pu = next_psf()
        for j in range(4):
            if g == 0:
                P.mm(pu[:, j * 4:(j + 1) * 4], c[:, 0, 512 + j * 128:512 + (j + 1) * 128], pc_[:, j * 4:(j + 1) * 4],
                     start=True, stop=False)
                P.mm(pu[:, j * 4:(j + 1) * 4], Vs[:, j * 128:(j + 1) * 128], pN[i][:, j * 4:(j + 1) * 4],
                     start=False, stop=True)
            else:
                for s in range(4):
                    col = j * 4 + s
                    P.mm(pu[:, col:col + 1], c[:, s, 512 + j * 128:512 + (j + 1) * 128], pc_[:, col:col + 1],
                         start=True, stop=False)
                    P.mm(pu[:, col:col + 1], Vs[:, j * 128:(j + 1) * 128], pN[i][:, col:col + 1],
                         start=False, stop=True)
        P.mm(pu[:, 16:32], ones[:, :], pc_[:, :], start=True, stop=False)
        P.mm(pu[:, 16:32], ones[0:64, :], pN[i][:, :], start=False, stop=True)
        uo = UT[:, :, 1024 + 4 * n:1024 + 4 * n + 4]
        lo = LT[:, :, 1024 + 4 * n:1024 + 4 * n + 4]
        pu_u = pu[:, 0:16].rearrange("p (j s) -> p j s", s=4)
        pu_l = pu[:, 16:32].rearrange("p (j s) -> p j s", s=4)
        if first:
            P.copy("act", uo, pu_u)
            P.copy("act", lo, pu_l)
        else:
            P.tt("dve", uo, pu_u, uo, ALU.add)
            P.tt("dve", lo, pu_l, lo, ALU.add)
    P.pop()


def attn_finish(P, nc, L, a):
    oT = L["oT"]
    UT, LT = a["UT"], a["LT"]
    P.op("dve", lambda e: e.reciprocal(out=LT[:, :, :], in_=LT[:, :, :]), [LT], [LT])
    P.tt("dve", oT[:, :, :], UT[:, :, :], LT[:, :, :], ALU.mult)
    if L["stage"] == 4:
        d_o = L["dout"]("d_oT", [128, 4, 1088], BF16)
        P.dma("sp", d_o[:, :, :], oT[:, :, :], oT)


def attn_host_inputs(m, inp, c):
    q = np.arange(128)
    m["maskA"] = (q[:, None] >= q[None, :]).astype(np.float32)
    m["maskB"] = (q[:, None] <= q[None, :]).astype(np.float32)
    s = np.arange(16) % 4
    m["maskC"] = (q[:, None] >= s[None, :]).astype(np.float32)
    k64 = np.arange(64)
    mn = np.zeros((64, 2, 16, 16), np.float32)
    for n in range(16):
        same = (k64 // 4 == n)
        mn[:, 0, n, :] = (same[:, None] & ((k64 % 4)[:, None] <= s[None, :])).astype(np.float32)
        mn[:, 1, n, :] = (same[:, None] & ((k64 % 4)[:, None] == s[None, :])).astype(np.float32)
    m["maskN"] = mn
    for g, d in enumerate((1, 4, 16)):
        pos = c * 1024 - 128 * d + np.arange(d)[None, :] + d * q[:, None]
        m[f"maskAv{g}"] = np.ascontiguousarray((pos >= 0).astype(np.float32)[:, :, None] * m["maskA"][:, None, :])
    m["cache0"] = np.asarray(inp["cache_kv_w128"])[0, 16 * c:16 * c + 16].reshape(16, 128, 1024)
    m["cache1"] = np.asarray(inp["cache_kv_w512"])[0, 16 * c:16 * c + 16].reshape(16, 512, 1024)
    m["cache2"] = np.asarray(inp["cache_kv_w2048"])[0, 16 * c:16 * c + 16].reshape(16, 2048, 1024)


def final_stage(P, nc, L):
    din, dout = L["din"], L["dout"]
    next_psf, next_psb = L["next_psf"], L["next_psb"]
    hT, oT, ygT, ident = L["hT"], L["oT"], L["G"]["ygT"], L["ident"]
    ada_chunk, norm_tile, mk_norm_bufs = L["ada_chunk"], L["norm_tile"], L["mk_norm_bufs"]
    w_in, x_own = L["w_in"], L["x_own"]
    D, KC, TO = 2048, 16, 1088
    OFF_G = 5632
    w_glu = din("w_glu", [1024, 4096])
    w_br = din("w_attn_br", [512, 2048])
    w_out = din("w_out", [2048, 2048])
    g2 = din("norm2_g", [1, D])
    w_rg = din("w_router_group", [D, 4])
    w_re = din("w_router_expert", [D, 16])
    b_r = din("b_router", [1, 20])
    w_gu = din("w_expert_gate_up", [16, D, 1024])
    w_dn = din("w_expert_down", [16, 512, D])
    selall_d = din("selall", [16, 16 * 128])
    y_o = dout("y_own", [TO, D])
    X1d = P.dram("X1d", [TO, D], F32)
    TB = ((0, 512), (512, 512), (1024, 64))

    def wload(dst, src2d, eng="pool"):
        P.dma(eng, dst, src2d.rearrange("(k p) n -> p k n", p=128), dst)

    P.push()
    gt1_p = P.sbuf("gt1_p", [128, D], F32)
    gt1_s = P.sbuf("gt1_s", [64, D], F32)
    ada_chunk(2, gt1_p, gt1_s)
    mixT = P.sbuf("mixT", [128, KC, TO], BF16)
    P.push()
    Wga = P.sbuf("Wga", [128, KC, 512], BF16)
    Wgb = P.sbuf("Wgb", [128, KC, 512], BF16)
    Wla = P.sbuf("Wla", [128, 8, 512], BF16)
    Wlb = P.sbuf("Wlb", [128, 8, 512], BF16)
    Wbr = P.sbuf("Wbr", [128, 4, 512], BF16)
    s1 = [P.sbuf(f"mx1{i}", [128, 512], F32) for i in range(2)]
    s2 = [P.sbuf(f"mx2{i}", [128, 512], F32) for i in range(2)]
    s3 = [P.sbuf(f"mx3{i}", [128, 512], F32) for i in range(2)]
    it = 0
    for cb in range(4):
        wload(Wga[:, :, :], w_in[:, OFF_G + cb * 512:OFF_G + (cb + 1) * 512])
        wload(Wgb[:, :, :], w_in[:, OFF_G + 2048 + cb * 512:OFF_G + 2048 + (cb + 1) * 512])
        wload(Wla[:, :, :], w_glu[:, cb * 512:(cb + 1) * 512])
        wload(Wlb[:, :, :], w_glu[:, 2048 + cb * 512:2048 + (cb + 1) * 512])
        wload(Wbr[:, :, :], w_br[:, cb * 512:(cb + 1) * 512])
        for i in range(4):
            cs_ = slice(i * 128, (i + 1) * 128)
            for (t0, n) in TB:
                k = it % 2
                it += 1
                pga = next_psf()
                for kc in range(KC):
                    P.mm(pga[:, 0:n], Wga[:, kc, cs_], hT[:, kc, t0:t0 + n], start=(kc == 0), stop=(kc == KC - 1))
                P.act(s1[k][:, 0:n], pga[:, 0:n], AF.Sigmoid)
                pgb = next_psf()
                for kc in range(KC):
                    P.mm(pgb[:, 0:n], Wgb[:, kc, cs_], hT[:, kc, t0:t0 + n], start=(kc == 0), stop=(kc == KC - 1))
                P.act(s2[k][:, 0:n], pgb[:, 0:n], AF.Sigmoid)
                pa2 = next_psf()
                for kc in range(8):
                    P.mm(pa2[:, 0:n], Wlb[:, kc, cs_], ygT[:, kc, t0:t0 + n], start=(kc == 0), stop=(kc == 7))
                P.act(s3[k][:, 0:n], pa2[:, 0:n], AF.Sigmoid)
                pa1 = next_psf()
                for kc in range(8):
                    P.mm(pa1[:, 0:n], Wla[:, kc, cs_], ygT[:, kc, t0:t0 + n], start=(kc == 0), stop=(kc == 7))
                P.tt("dve", s3[k][:, 0:n], pa1[:, 0:n], s3[k][:, 0:n], ALU.mult)
                P.tt("pool", s1[k][:, 0:n], s1[k][:, 0:n], s3[k][:, 0:n], ALU.mult)
                pbb = next_psf()
                for kc in range(4):
                    P.mm(pbb[:, 0:n], Wbr[:, kc, cs_], oT[:, kc, t0:t0 + n], start=(kc == 0), stop=(kc == 3))
                P.tt("dve", s2[k][:, 0:n], pbb[:, 0:n], s2[k][:, 0:n], ALU.mult)
                P.tt("pool", mixT[:, cb * 4 + i, t0:t0 + n], s1[k][:, 0:n], s2[k][:, 0:n], ALU.add)
    P.pop()

    P.push()
    Wo = P.sbuf("Wo", [128, KC, 512], BF16)
    xq = [P.sbuf(f"xq{i}", [128, 512], F32) for i in range(2)]
    for cb in range(4):
        wload(Wo[:, :, :], w_out[:, cb * 512:(cb + 1) * 512])
        for t in range(9):
            R = 128 if t < 8 else 64
            gt = gt1_p if t < 8 else gt1_s
            x = xq[(cb * 9 + t) % 2]
            P.dma("sp", x[0:R, :], x_own[t * 128:t * 128 + R, cb * 512:(cb + 1) * 512], x)
            ps = next_psf()
            for kc in range(KC):
                P.mm(ps[0:R, :], mixT[:, kc, t * 128:t * 128 + R], Wo[:, kc, :], start=(kc == 0), stop=(kc == KC - 1))
            P.tt("dve", ps[0:R, :], ps[0:R, :], gt[0:R, cb * 512:(cb + 1) * 512], ALU.mult)
            P.tt("dve", x[0:R, :], x[0:R, :], ps[0:R, :], ALU.add)
            P.dma("sp", X1d[t * 128:t * 128 + R, cb * 512:(cb + 1) * 512], x[0:R, :], x)
    P.pop()
    P.pop()

    P.push()
    sh2_p = P.sbuf("sh2_p", [128, D], F32)
    sh2_s = P.sbuf("sh2_s", [64, D], F32)
    A2_p = P.sbuf("A2_p", [128, D], F32)
    A2_s = P.sbuf("A2_s", [64, D], F32)
    g2_bc = P.sbuf("g2_bc", [128, D], F32)
    P.dma("sp", g2_bc[:, :], g2[0:1, :].to_broadcast([128, D]), g2_bc)
    ada_chunk(3, sh2_p, sh2_s)
    ada_chunk(4, A2_p, A2_s)
    P.stt("dve", A2_p[:, :], A2_p[:, :], 1.0, g2_bc[:, :], ALU.add, ALU.mult)
    P.stt("dve", A2_s[:, :], A2_s[:, :], 1.0, g2_bc[0:64, :], ALU.add, ALU.mult)
    P.push()
    mk_norm_bufs(2)
    for t in range(9):
        R = 128 if t < 8 else 64
        A_t, sh_t = (A2_p, sh2_p) if t < 8 else (A2_s, sh2_s)
        norm_tile(X1d[t * 128:t * 128 + R, :], R, A_t, sh_t,
                  lambda kc0, t=t, R=R: hT[:, kc0:kc0 + 8, t * 128:t * 128 + R])
    P.pop()
    P.pop()
    h2T = hT

    P.push()
    Wr = P.sbuf("Wr", [128, KC, 20], BF16)
    P.dma("pool", Wr[:, :, 0:4], w_rg[:, :].rearrange("(k p) n -> p k n", p=128), Wr)
    P.dma("pool", Wr[:, :, 4:20], w_re[:, :].rearrange("(k p) n -> p k n", p=128), Wr)
    br_bc = P.sbuf("br_bc", [128, 20], F32)
    P.dma("sp", br_bc[:, :], b_r[0:1, :].to_broadcast([128, 20]), br_bc)
    combT = P.sbuf("combT", [16, TO], F32)
    selall = P.sbuf("selall", [16, 16 * 128], F32)
    P.dma("sp", selall[:, :], selall_d[:, :], selall)
    identf = P.sbuf("identf2", [128, 128], F32)
    P.dma("sp", identf[:, :], L["ident_d"][:, :], identf)
    P.push()
    rb = [P.sbuf(f"rb{i}", [128, 80], F32) for i in range(2)]
    for t in range(9):
        R = 128 if t < 8 else 64
        w = rb[t % 2]
        lg, le = w[0:R, 0:4], w[0:R, 4:20]
        mx, oh, eg, sm, sel, k1, k2, m1, m2, sel2, c4 = (w[0:R, 20:21], w[0:R, 21:25], w[0:R, 25:29], w[0:R, 29:30],
                                                         w[0:R, 30:34], w[0:R, 34:38], w[0:R, 38:42], w[0:R, 42:43],
                                                         w[0:R, 43:44], w[0:R, 44:48], w[0:R, 48:52])
        ps = next_psf()
        for kc in range(KC):
            P.mm(ps[0:R, 0:20], h2T[:, kc, t * 128:t * 128 + R], Wr[:, kc, :], start=(kc == 0), stop=(kc == KC - 1))
        P.tt("dve", w[0:R, 0:20], ps[0:R, 0:20], br_bc[0:R, :], ALU.add)
        P.op("dve", lambda e, o=mx, i_=lg: e.tensor_reduce(out=o, in_=i_, axis=AX.X, op=ALU.max), [w], [w])
        P.ts("dve", oh, lg, mx, ALU.is_equal)
        P.ts("dve", eg, lg, mx, ALU.subtract)
        P.act(eg, eg, AF.Exp)
        P.op("dve", lambda e, o=sm, i_=eg: e.tensor_reduce(out=o, in_=i_, axis=AX.X, op=ALU.add), [w], [w])
        P.op("dve", lambda e, o=sm: e.reciprocal(out=o, in_=o), [w], [w])
        t16 = w[0:R, 52:68]
        P.tt("dve", t16.rearrange("p (g e) -> p g e", e=4), le.rearrange("p (g e) -> p g e", e=4),
             oh.unsqueeze(2).to_broadcast([R, 4, 4]), ALU.mult)
        P.op("dve", lambda e, o=sel, i_=t16.rearrange("p (g e) -> p e g", e=4): e.tensor_reduce(
            out=o, in_=i_, axis=AX.X, op=ALU.add), [w], [w])
        P.op("dve", lambda e, o=m1, i_=sel: e.tensor_reduce(out=o, in_=i_, axis=AX.X, op=ALU.max), [w], [w])
        P.ts("dve", k1, sel, m1, ALU.is_equal)
        P.stt("dve", sel2, k1, -1.0e30, sel, ALU.mult, ALU.add)
        P.op("dve", lambda e, o=m2, i_=sel2: e.tensor_reduce(out=o, in_=i_, axis=AX.X, op=ALU.max), [w], [w])
        P.ts("dve", k2, sel2, m2, ALU.is_equal)
        P.tt("dve", m2, m2, m1, ALU.subtract)
        P.act(m2, m2, AF.Exp)
        P.ts("dve", m1, m2, 1.0, ALU.add)
        P.op("dve", lambda e, o=m1: e.reciprocal(out=o, in_=o), [w], [w])
        P.tt("dve", m2, m2, m1, ALU.mult)
        P.ts("dve", k1, k1, m1, ALU.mult)
        P.ts("dve", k2, k2, m2, ALU.mult)
        P.tt("dve", c4, k1, k2, ALU.add)
        P.ts("dve", c4, c4, sm, ALU.mult)
        P.tt("dve", t16.rearrange("p (g e) -> p g e", e=4), oh.unsqueeze(2).to_broadcast([R, 4, 4]),
             c4.unsqueeze(1).to_broadcast([R, 4, 4]), ALU.mult)
        pt = next_psf()
        P.op("pe", lambda e, o=pt[0:16, 0:R], i_=t16, idn=identf[0:R, 0:R]: e.transpose(o, i_, idn), [w, identf], [pt])
        P.copy("dve", combT[:, t * 128:t * 128 + R], pt[0:16, 0:R])
    P.pop()

    gt2_p = L["A1_p"]
    gt2_s = L["sh1_p"]
    ada_chunk(5, gt2_p, gt2_s)
    ygf = ygT[:, :, :].rearrange("p a b -> p (a b)").bitcast(F32)
    Wg = P.sbuf("Wg", [128, KC, 512], BF16)
    Wu_ = P.sbuf("Wu_", [128, KC, 512], BF16)
    Wd = P.sbuf("Wd", [128, 4, D], BF16)
    actT = P.sbuf("actT", [128, 4, 640], BF16)
    sgl = [P.sbuf(f"sgl{i}", [128, 512], F32) for i in range(2)]
    for (tile0, ntile) in ((0, 5), (5, 4)):
        tok0 = tile0 * 128
        ntok = min(ntile * 128, TO - tok0)
        tbs = [(o, min(512, ntok - o)) for o in range(0, ntok, 512)]
        P.push()
        acc = P.sbuf("moe_acc", [128, ntile, D], F32)
        for e in range(16):
            wload(Wg[:, :, :], w_gu[e, :, 0:512])
            wload(Wu_[:, :, :], w_gu[e, :, 512:1024])
            P.dma("pool", Wd[:, :, :], w_dn[e, :, :].rearrange("(k p) n -> p k n", p=128), Wd)
            for (o, n) in tbs:
                pc_ = next_psf()
                P.mm(pc_[:, 0:n], selall[:, e * 128:(e + 1) * 128], combT[:, tok0 + o:tok0 + o + n], start=True, stop=True)
                cbs = sgl[1]
                P.copy("act", cbs[:, 0:n], pc_[:, 0:n])
                for fc in range(4):
                    pg = next_psf()
                    for kc in range(KC):
                        P.mm(pg[:, 0:n], Wg[:, kc, fc * 128:(fc + 1) * 128], h2T[:, kc, tok0 + o:tok0 + o + n],
                             start=(kc == 0), stop=(kc == KC - 1))
                    pu = next_psf()
                    for kc in range(KC):
                        P.mm(pu[:, 0:n], Wu_[:, kc, fc * 128:(fc + 1) * 128], h2T[:, kc, tok0 + o:tok0 + o + n],
                             start=(kc == 0), stop=(kc == KC - 1))
                    sg = sgl[0]
                    P.act(sg[:, 0:n], pg[:, 0:n], AF.Silu)
                    P.tt("dve", sg[:, 0:n], sg[:, 0:n], pu[:, 0:n], ALU.mult)
                    P.tt("pool", actT[:, fc, o:o + n], sg[:, 0:n], cbs[:, 0:n], ALU.mult)
            for tt_ in range(ntile):
                R = min(128, ntok - tt_ * 128)
                for db in range(4):
                    pd = next_psf()
                    for fc in range(4):
                        P.mm(pd[0:R, :], actT[:, fc, tt_ * 128:tt_ * 128 + R], Wd[:, fc, db * 512:(db + 1) * 512],
                             start=(fc == 0), stop=(fc == 3))
                    dst = acc[0:R, tt_, db * 512:(db + 1) * 512]
                    if e == 0:
                        P.copy("act", dst, pd[0:R, :])
                    else:
                        P.tt("dve", dst, pd[0:R, :], dst, ALU.add)
        xr = [ygf[:, 0:D], ygf[:, D:2 * D]]
        for tt_ in range(ntile):
            t = tile0 + tt_
            R = min(128, TO - t * 128)
            gt = gt2_p if t < 8 else gt2_s
            x = xr[tt_ % 2]
            P.dma("sp", x[0:R, :], X1d[t * 128:t * 128 + R, :], ygT)
            P.tt("dve", acc[0:R, tt_, :], acc[0:R, tt_, :], gt[0:R, :], ALU.mult)
            P.tt("pool", x[0:R, :], x[0:R, :], acc[0:R, tt_, :], ALU.add)
            P.dma("sp", y_o[t * 128:t * 128 + R, :], x[0:R, :], ygT)
        P.pop()
    P.pop()


def final_host_inputs(m, inp, c):
    m["w_glu"] = np.asarray(inp["w_glu"])[0]
    m["w_attn_br"] = np.asarray(inp["w_attn_br"])[0]
    m["w_out"] = np.asarray(inp["w_out"])[0]
    m["norm2_g"] = np.asarray(inp["norm2_g"])
    m["w_router_group"] = np.asarray(inp["w_router_group"])[0]
    m["w_router_expert"] = np.asarray(inp["w_router_expert"])[0]
    m["b_router"] = np.ascontiguousarray(np.concatenate([np.asarray(inp["b_router_group"]), np.asarray(inp["b_router_expert"])], 1))
    m["w_expert_gate_up"] = np.asarray(inp["w_expert_gate_up"])[0]
    m["w_expert_down"] = np.asarray(inp["w_expert_down"])[0]
    sel = np.zeros((16, 16, 128), np.float32)
    for e in range(16):
        sel[e, e, :] = 1.0
    m["selall"] = sel.reshape(16, 16 * 128)

import numpy as np
from concourse.bass_utils import run_bass_kernel_spmd

NCORES = 8
D = 2048
KC = 16
TP = 1024
TS = 64
TO = TP + TS
HALO = 2048
DIL = (1, 4, 16)
EPS = 1e-6
OFF_Q, OFF_K, OFF_V, OFF_G = 1024, 2560, 4096, 5632
STAGE = 1


def build(stage=STAGE):
    stage = int(stage)
    nc = bass.Bass("TRN2", target_bir_lowering=False)
    es = ExitStack()
    P = Prog(nc, es)

    def din(name, shape, dt=F32):
        return P.dram(name, shape, dt, kind="ExternalInput")

    def dout(name, shape, dt=F32):
        return P.dram(name, shape, dt, kind="ExternalOutput")

    x_own = din("x_own", [TO, D])
    x_halo = din("x_halo", [HALO, D])
    c_all = din("c_all", [17, D])
    w_ada = din("w_ada", [D, 6 * D])
    b_ada = din("b_ada", [1, 6 * D])
    g1 = din("norm1_g", [1, D])
    w_in = din("w_in", [D, 9728])
    qg = din("q_norm_g", [1, 128])
    kg = din("k_norm_g", [1, 128])
    ident_d = din("ident", [128, 128])
    cos_own_d = din("cos_own", [TO, 16])
    sin_own_d = din("sin_own", [TO, 16])
    cos_halo_d = din("cos_halo", [HALO, 16])
    sin_halo_d = din("sin_halo", [HALO, 16])
    valid_halo_d = din("valid_halo", [128, 16])

    kvp = [dout(f"kvp{g}", [TP, 1024]) for g in range(3)]
    kvs = [dout(f"kvs{g}", [TS, 1024]) for g in range(3)]

    Vd = [P.dram(f"Vd{g}", [128 * DIL[g] + TP, 512], BF16) for g in range(3)]

    psf = [P.psum(f"psf{i}", [128, 512], F32) for i in range(5)]
    psx = P.psum("psx", [128, 512], F32)
    psb = [P.psum(f"psb{i}", [128, 1024], BF16) for i in range(2)]
    cnt = {"f": 0, "b": 0}

    def next_psf():
        cnt["f"] += 1
        return psf[cnt["f"] % len(psf)]

    def next_psb():
        cnt["b"] += 1
        return psb[cnt["b"] % len(psb)]

    ident = P.sbuf("ident_sb", [128, 128], BF16)
    P.dma("pool", ident[:, :], ident_d[:, :], ident)
    cos_own = P.sbuf("cos_own_sb", [128, 9, 16], F32)
    sin_own = P.sbuf("sin_own_sb", [128, 9, 16], F32)
    cos_halo = P.sbuf("cos_halo_sb", [128, 16, 16], F32)
    sin_halo = P.sbuf("sin_halo_sb", [128, 16, 16], F32)
    valid_halo = P.sbuf("valid_halo_sb", [128, 16], F32)
    for (sb, dr) in ((cos_own, cos_own_d), (sin_own, sin_own_d)):
        P.dma("sp", sb[:, 0:8, :], dr[0:TP, :].rearrange("(t p) f -> p t f", p=128), sb)
        P.dma("sp", sb[0:64, 8, :], dr[TP:TO, :], sb)
    for (sb, dr) in ((cos_halo, cos_halo_d), (sin_halo, sin_halo_d)):
        P.dma("sp", sb[:, :, :], dr[:, :].rearrange("(t p) f -> p t f", p=128), sb)
    P.dma("sp", valid_halo[:, :], valid_halo_d[:, :], valid_halo)
    qg_bc = P.sbuf("qg_bc", [128, 128], F32)
    kg_bc = P.sbuf("kg_bc", [128, 128], F32)
    P.dma("sp", qg_bc[:, :], qg[0:1, :].to_broadcast([128, 128]), qg_bc)
    P.dma("sp", kg_bc[:, :], kg[0:1, :].to_broadcast([128, 128]), kg_bc)
    lhsT_p = P.sbuf("lhsT_p", [128, KC, 128], BF16)
    lhsT_s = P.sbuf("lhsT_s", [128, KC, 64], BF16)
    sh1_p = P.sbuf("sh1_p", [128, D], F32)
    A1_p = P.sbuf("A1_p", [128, D], F32)
    hT = P.sbuf("hT", [128, KC, TO], BF16)
    P.push()
    sh1_s = P.sbuf("sh1_s", [64, D], F32)
    A1_s = P.sbuf("A1_s", [64, D], F32)

    P.push()
    g1_bc = P.sbuf("g1_bc", [128, D], F32)
    P.dma("sp", g1_bc[:, :], g1[0:1, :].to_broadcast([128, D]), g1_bc)
    c_sb = P.sbuf("c_sb", [17, D], F32)
    P.dma("sp", c_sb[:, :], c_all[:, :], c_sb)
    c_bf = P.sbuf("c_bf", [17, D], BF16)
    P.act(c_bf[:, :], c_sb[:, :], AF.Silu)
    sT = P.sbuf("sT", [128, KC, 17], BF16)
    pt = next_psb()
    for kc in range(KC):
        P.transpose(pt[:, kc * 32:kc * 32 + 17], c_bf[0:17, kc * 128:(kc + 1) * 128], ident[0:17, 0:17])
    P.copy("dve", sT[:, :, :], pt[:, 0:KC * 32].rearrange("p (k n) -> p k n", n=32)[:, :, 0:17])
    P.copy("dve", lhsT_p[:, :, :], sT[:, :, 0:1].to_broadcast([128, KC, 128]))
    P.copy("dve", lhsT_s[:, :, :].rearrange("p k (n s) -> p k n s", s=4),
           sT[:, :, 1:17].unsqueeze(3).to_broadcast([128, KC, 16, 4]))

    wcnt = {"a": 0}

    def ada_chunk(i, dst_p, dst_s):
        P.push()
        wA = [P.sbuf(f"wA{i}", [128, KC, 512], BF16) for i in range(2)]
        bias_bc = P.sbuf("bias_bc", [128, D], F32)
        P.dma("sp", bias_bc[:, :], b_ada[0:1, i * D:(i + 1) * D].to_broadcast([128, D]), bias_bc)
        for nb in range(4):
            w = wA[wcnt["a"] % 2]
            wcnt["a"] += 1
            c0 = i * D + nb * 512
            P.dma("pool", w[:, :, :], w_ada[:, c0:c0 + 512].rearrange("(k p) n -> p k n", p=128), w)
            pp = next_psf()
            for kc in range(KC):
                P.mm(pp[:, :], lhsT_p[:, kc, :], w[:, kc, :], start=(kc == 0), stop=(kc == KC - 1))
            P.tt("dve", dst_p[:, nb * 512:(nb + 1) * 512], pp[:, :], bias_bc[:, nb * 512:(nb + 1) * 512], ALU.add)
            pq = next_psf()
            for kc in range(KC):
                P.mm(pq[0:64, :], lhsT_s[:, kc, :], w[:, kc, :], start=(kc == 0), stop=(kc == KC - 1))
            P.tt("dve", dst_s[0:64, nb * 512:(nb + 1) * 512], pq[0:64, :], bias_bc[0:64, nb * 512:(nb + 1) * 512],
                 ALU.add)
        P.pop()

    ada_chunk(0, sh1_p, sh1_s)
    ada_chunk(1, A1_p, A1_s)
    P.stt("dve", A1_p[:, :], A1_p[:, :], 1.0, g1_bc[:, :], ALU.add, ALU.mult)
    P.stt("dve", A1_s[:, :], A1_s[:, :], 1.0, g1_bc[0:64, :], ALU.add, ALU.mult)
    P.pop()

    P.push()
    nb_ = {}

    def mk_norm_bufs(n=2):
        nb_["xt"] = [P.sbuf(f"xt{i}", [128, D], F32) for i in range(n)]
        nb_["hb"] = [P.sbuf(f"hb{i}", [128, D], BF16) for i in range(n)]
        nb_["st"] = [P.sbuf(f"nst{i}", [128, 2], F32) for i in range(n)]

    mk_norm_bufs()
    ncnt = {"n": 0}

    def norm_tile(src_ap, R, A_t, sh_t, dst_fn):
        i = ncnt["n"] % len(nb_["xt"])
        ncnt["n"] += 1
        x = nb_["xt"][i]
        st = nb_["st"][i]
        h = nb_["hb"][i]
        P.dma("sp", x[0:R, :], src_ap, x)
        P.memset("dve", st[0:R, 0:1], 0.0)
        P.act(h[0:R, :], x[0:R, :], AF.Square, accum_out=st[0:R, 0:1])
        P.ts("dve", st[0:R, 1:2], st[0:R, 0:1], 1.0 / D, ALU.mult, EPS, ALU.add)
        P.act(st[0:R, 1:2], st[0:R, 1:2], AF.Sqrt)
        P.op("dve", lambda e, o=st[0:R, 1:2]: e.reciprocal(out=o, in_=o), [st], [st])
        P.stt("dve", x[0:R, :], x[0:R, :], st[0:R, 1:2], A_t[0:R, :], ALU.mult, ALU.mult)
        P.tt("dve", h[0:R, :], x[0:R, :], sh_t[0:R, :], ALU.add)
        for half in range(2):
            pt = next_psb()
            for k in range(8):
                kc = half * 8 + k
                P.transpose(pt[:, k * 128:k * 128 + R], h[0:R, kc * 128:(kc + 1) * 128], ident[0:R, 0:R])
            P.copy("act", dst_fn(half * 8), pt[:, :].rearrange("p (k n) -> p k n", n=128)[:, :, 0:R])

    for t in range(9):
        R = 128 if t < 8 else 64
        A_t, sh_t = (A1_p, sh1_p) if t < 8 else (A1_s, sh1_s)
        norm_tile(x_own[t * 128:t * 128 + R, :], R, A_t, sh_t,
                  lambda kc0, t=t, R=R: hT[:, kc0:kc0 + 8, t * 128:t * 128 + R])

    P.pop()
    P.pop()
    oT = P.sbuf("oT", [128, 4, TO], BF16)
    P.push()
    KT1 = P.sbuf("KT", [128, 4, 128 * 16 + TP], BF16)
    QT1 = P.sbuf("QT", [128, 4, TP], BF16)
    KT = [KT1] * 3
    QT = [QT1] * 3
    KTs = [P.sbuf(f"KTs{g}", [128, 4, TS], BF16) for g in range(3)]
    QTs = [P.sbuf(f"QTs{g}", [128, 4, TS], BF16) for g in range(3)]
    Vs = [P.sbuf(f"Vs{g}", [TS, 512], BF16) for g in range(3)]
    att = attn_setup(P, nc, locals()) if stage >= 4 else None

    def proj_group(g):
        P.push()
        mk_norm_bufs(1)
        wK = P.sbuf("wK", [128, KC, 512], BF16)
        wV = P.sbuf("wV", [128, KC, 512], BF16)
        wQ = wK
        hTh = [P.sbuf(f"hTh{i}", [128, KC, 128], BF16) for i in range(1)] * 2
        knf = [P.sbuf(f"knf{i}", [128, 512], F32) for i in range(1)] * 2
        vf = [P.sbuf(f"vf{i}", [128, 512], F32) for i in range(1)] * 2
        kb = [P.sbuf(f"kb{i}", [128, 512], BF16) for i in range(1)] * 2
        vb = [P.sbuf(f"vb{i}", [128, 512], BF16) for i in range(1)] * 2
        qst = [P.sbuf(f"qst{i}", [128, 8], F32) for i in range(2)]
        rt = [P.sbuf(f"rt{i}", [128, 4, 4, 16], F32) for i in range(1)] * 2
        junk2 = P.sbuf("junk2", [128, 128], BF16)
        pc = {"n": 0}

        def qk_post(ps, R, g_bc, cos_ap, sin_ap, out_f32):
            i = pc["n"] % 2
            pc["n"] += 1
            st = qst[i]
            r = rt[i]
            P.memset("dve", st[0:R, 0:4], 0.0)
            for j in range(4):
                P.act(junk2[0:R, :], ps[0:R, j * 128:(j + 1) * 128], AF.Square, accum_out=st[0:R, j:j + 1])
            P.ts("dve", st[0:R, 4:8], st[0:R, 0:4], 1.0 / 128, ALU.mult, EPS, ALU.add)
            P.act(st[0:R, 4:8], st[0:R, 4:8], AF.Sqrt)
            P.op("dve", lambda e, o=st[0:R, 4:8]: e.reciprocal(out=o, in_=o), [st], [st])
            for j in range(4):
                P.stt("dve", out_f32[0:R, j * 128:(j + 1) * 128], ps[0:R, j * 128:(j + 1) * 128],
                      st[0:R, 4 + j:5 + j], g_bc[0:R, :], ALU.mult, ALU.mult)
            o3 = out_f32[0:R, :].rearrange("p (j d) -> p j d", d=128)
            x1 = o3[:, :, 0:16]
            x2 = o3[:, :, 16:32]
            cb = cos_ap.unsqueeze(1).to_broadcast([R, 4, 16])
            sb_ = sin_ap.unsqueeze(1).to_broadcast([R, 4, 16])
            P.tt("pool", r[0:R, 0], x1, cb, ALU.mult)
            P.tt("pool", r[0:R, 1], x2, sb_, ALU.mult)
            P.tt("pool", r[0:R, 2], x2, cb, ALU.mult)
            P.tt("pool", r[0:R, 3], x1, sb_, ALU.mult)
            P.tt("pool", x1, r[0:R, 0], r[0:R, 1], ALU.subtract)
            P.tt("pool", x2, r[0:R, 2], r[0:R, 3], ALU.add)

        def to_featmajor(src_bf, R, dst_ap):
            pt = next_psb()
            for j in range(4):
                P.transpose(pt[:, j * 128:j * 128 + R], src_bf[0:R, j * 128:(j + 1) * 128], ident[0:R, 0:R])
            P.copy("act", dst_ap, pt[:, 0:512].rearrange("p (j n) -> p j n", n=128)[:, :, 0:R])

        d = DIL[g]
        nh = d
        P.dma("pool", wK[:, :, :], w_in[:, OFF_K + g * 512:OFF_K + (g + 1) * 512].rearrange("(k p) n -> p k n", p=128), wK)
        P.dma("pool", wV[:, :, :], w_in[:, OFF_V + g * 512:OFF_V + (g + 1) * 512].rearrange("(k p) n -> p k n", p=128), wV)
        tiles = [("h", 16 - nh + i) for i in range(nh)] + [("o", t) for t in range(9)]
        for (kind, t) in tiles:
            if kind == "h":
                R = 128
                hh = hTh[t % 2]
                norm_tile(x_halo[t * 128:(t + 1) * 128, :], 128, A1_p, sh1_p,
                          lambda kc0, hh=hh: hh[:, kc0:kc0 + 8, :])
                lh = lambda kc: hh[:, kc, :]
                cos_ap, sin_ap = cos_halo[:, t, :], sin_halo[:, t, :]
                kk0 = (t - (16 - nh)) * 128
            else:
                R = 128 if t < 8 else 64
                lh = lambda kc, t=t, R=R: hT[:, kc, t * 128:t * 128 + R]
                cos_ap, sin_ap = cos_own[0:R, t, :], sin_own[0:R, t, :]
                kk0 = nh * 128 + t * 128
            i = pc["n"] % 2
            pk = next_psf()
            for kc in range(KC):
                P.mm(pk[0:R, :], lh(kc), wK[:, kc, :], start=(kc == 0), stop=(kc == KC - 1))
            pv = next_psf()
            for kc in range(KC):
                P.mm(pv[0:R, :], lh(kc), wV[:, kc, :], start=(kc == 0), stop=(kc == KC - 1))
            kf_t, vf_t, kb_t, vb_t = knf[i], vf[i], kb[i], vb[i]
            qk_post(pk, R, kg_bc, cos_ap, sin_ap, kf_t)
            P.copy("dve", kb_t[0:R, :], kf_t[0:R, :])
            if kind == "h":
                P.act(vb_t[0:R, :], pv[0:R, :], AF.Copy, scale=valid_halo[0:R, t:t + 1])
                to_featmajor(kb_t, R, KT[g][:, :, kk0:kk0 + R])
                P.dma("sp", Vd[g][kk0:kk0 + R, :], vb_t[0:R, :], vb_t)
            else:
                P.copy("act", vf_t[0:R, :], pv[0:R, :])
                P.copy("act", vb_t[0:R, :], pv[0:R, :])
                if t < 8:
                    to_featmajor(kb_t, R, KT[g][:, :, kk0:kk0 + R])
                    P.dma("sp", Vd[g][kk0:kk0 + R, :], vb_t[0:R, :], vb_t)
                    P.dma("sp", kvp[g][t * 128:t * 128 + R, 0:512], kf_t[0:R, :], kf_t)
                    P.dma("sp", kvp[g][t * 128:t * 128 + R, 512:1024], vf_t[0:R, :], vf_t)
                else:
                    to_featmajor(kb_t, R, KTs[g][:, :, 0:R])
                    P.copy("pool", Vs[g][0:R, :], vb_t[0:R, :])
                    P.dma("sp", kvs[g][0:R, 0:512], kf_t[0:R, :], kf_t)
                    P.dma("sp", kvs[g][0:R, 512:1024], vf_t[0:R, :], vf_t)
        P.dma("pool", wQ[:, :, :], w_in[:, OFF_Q + g * 512:OFF_Q + (g + 1) * 512].rearrange("(k p) n -> p k n", p=128), wQ)
        for t in range(9):
            R = 128 if t < 8 else 64
            cos_ap, sin_ap = cos_own[0:R, t, :], sin_own[0:R, t, :]
            pq = next_psf()
            for kc in range(KC):
                P.mm(pq[0:R, :], hT[:, kc, t * 128:t * 128 + R], wQ[:, kc, :], start=(kc == 0), stop=(kc == KC - 1))
            i2 = pc["n"] % 2
            qf_t, qb_t = knf[i2], kb[i2]
            qk_post(pq, R, qg_bc, cos_ap, sin_ap, qf_t)
            P.copy("dve", qb_t[0:R, :], qf_t[0:R, :])
            if t < 8:
                to_featmajor(qb_t, R, QT[g][:, :, t * 128:t * 128 + R])
            else:
                to_featmajor(qb_t, R, QTs[g][:, :, 0:R])


        P.pop()

    for g in range(3):
        proj_group(g)
        if stage >= 4:
            attn_group(P, nc, locals(), att, g)
    if stage >= 4:
        attn_finish(P, nc, locals(), att)
    P.pop()


    G = {}
    import os as _os
    if stage >= 2 and not _os.environ.get("SKIP_SSM"):
        ssm_stage(P, nc, locals())
    if stage >= 5:
        final_stage(P, nc, locals())
    elif stage == 3:
        y_o = dout("y_own", [TO, D])
        P.push()
        yt = [P.sbuf(f"ypass{i}", [128, D], F32) for i in range(2)]
        for t in range(9):
            R = 128 if t < 8 else 64
            P.dma("sp", yt[t % 2][0:R, :], x_own[t * 128:t * 128 + R, :], yt[t % 2])
            P.dma("sp", y_o[t * 128:t * 128 + R, :], yt[t % 2][0:R, :], yt[t % 2])
        P.pop()

    P.finish()
    stats = P.emit()
    return nc, es, stats


def rope_tables(pos):
    half = 16
    inv_freq = (np.float32(500000.0) ** (-np.arange(half, dtype=np.float32) / np.float32(half))).astype(np.float32)
    ang = pos.astype(np.float32)[:, None] * inv_freq[None, :]
    return np.cos(ang).astype(np.float32), np.sin(ang).astype(np.float32)


def make_in_maps(inp):
    xp = np.asarray(inp["x_prompt"])[0]
    xs = np.asarray(inp["x_sample"])
    maps = []
    ident = np.eye(128, dtype=np.float32)
    for c in range(NCORES):
        m = {}
        m["x_own"] = np.ascontiguousarray(np.concatenate(
            [xp[c * TP:(c + 1) * TP], xs[16 * c:16 * c + 16].reshape(TS, D)], 0))
        lo = c * TP - HALO
        xh = np.zeros((HALO, D), np.float32)
        if lo < 0:
            if c > 0:
                xh[-lo:] = xp[0:c * TP]
        else:
            xh[:] = xp[lo:c * TP]
        m["x_halo"] = xh
        m["c_all"] = np.ascontiguousarray(np.concatenate([np.asarray(inp["c_prompt"]), np.asarray(inp["c_sample"])[16 * c:16 * c + 16]], 0))
        m["w_ada"] = np.asarray(inp["w_ada"])[0]
        m["b_ada"] = np.asarray(inp["b_ada"])
        m["norm1_g"] = np.asarray(inp["norm1_g"])
        m["w_in"] = np.asarray(inp["w_in"])[0]
        m["q_norm_g"] = np.asarray(inp["q_norm_g"])
        m["k_norm_g"] = np.asarray(inp["k_norm_g"])
        m["ident"] = ident
        pos_own = np.concatenate([np.arange(c * TP, (c + 1) * TP), np.tile(2048 + np.arange(4), 16)])
        co, so = rope_tables(pos_own)
        m["cos_own"], m["sin_own"] = co, so
        pos_h = np.arange(lo, c * TP)
        ch, sh = rope_tables(np.maximum(pos_h, 0))
        m["cos_halo"], m["sin_halo"] = ch, sh
        ssm_host_inputs(m, inp, c)
        attn_host_inputs(m, inp, c)
        final_host_inputs(m, inp, c)
        m["valid_halo"] = np.ascontiguousarray((pos_h >= 0).astype(np.float32).reshape(16, 128).T)
        maps.append(m)
    return maps


_CACHE = {}


def run(inp, stage=STAGE):
    if stage not in _CACHE:
        _CACHE[stage] = build(stage)
    nc, es, stats = _CACHE[stage]
    maps = make_in_maps(inp)
    res = run_bass_kernel_spmd(nc, maps, core_ids=list(range(NCORES)))
    return res.results


def ssm_host_inputs(m, inp, c):
    are = np.asarray(inp["ssm_a_re"])[0]
    aim = np.asarray(inp["ssm_a_im"])[0]
    ldt = np.asarray(inp["ssm_log_dt"])[0]
    m["x_all"] = np.asarray(inp["x_prompt"])[0]
    m["are_row"] = np.ascontiguousarray(are.reshape(1, 4096))
    m["aim_row"] = np.ascontiguousarray(aim.reshape(1, 4096))
    m["ldt_row"] = np.ascontiguousarray(np.repeat(ldt, 64).reshape(1, 4096))
    m["are_T2"] = np.ascontiguousarray(np.tile(are.T, (2, 1)))
    m["aim_T2"] = np.ascontiguousarray(np.tile(aim.T, (2, 1)))
    m["ldt_bc"] = np.ascontiguousarray(np.tile(ldt[None, :], (128, 1)))
    bre = np.asarray(inp["ssm_b_re"])[0]
    bim = np.asarray(inp["ssm_b_im"])[0]
    m["b_T"] = np.ascontiguousarray(np.stack([bre, bim], 0).transpose(3, 0, 1, 2).reshape(16, 2, 4096))
    cre = np.asarray(inp["ssm_c_re"])[0]
    cim = np.asarray(inp["ssm_c_im"])[0]
    m["c_T"] = np.ascontiguousarray(np.concatenate([cre.transpose(2, 0, 1), cim.transpose(2, 0, 1)], 0))
    m["c_T2"] = np.ascontiguousarray(np.concatenate([cim.transpose(2, 0, 1), cre.transpose(2, 0, 1)], 0))
    m["d_T"] = np.ascontiguousarray(np.asarray(inp["ssm_d"])[0].reshape(8, 128).T)
    q = np.arange(128)
    m["tau"] = np.stack([q + 1, -(q + 1), (q % 4) + 1, -((q % 4) + 1)], 1).astype(np.float32)
    m["tri"] = (q[:, None] <= q[None, :]).astype(np.float32)
    q64 = np.arange(64)
    m["tris"] = ((q64[:, None] <= q64[None, :]) & (q64[:, None] // 4 == q64[None, :] // 4)).astype(np.float32)
    m["sels"] = (np.arange(16)[:, None] == q64[None, :] // 4).astype(np.float32)
    m["swapm"] = (q[:, None] == (q[None, :] + 64) % 128).astype(np.float32)
    oh = np.zeros((64, 8), np.float32)
    for i in range(8):
        oh[8 * c + i, i] = 1.0
    m["onehot"] = np.ascontiguousarray(np.tile(oh[None], (128, 1, 1)))
    sre = np.asarray(inp["state_ssm_re"])[0, 16 * c:16 * c + 16]
    sim = np.asarray(inp["state_ssm_im"])[0, 16 * c:16 * c + 16]
    m["s0"] = np.ascontiguousarray(np.repeat(np.stack([sre, sim], 2).reshape(16, 8192), 4, axis=0))


def assemble(res):
    f32 = np.float32
    y_p = np.concatenate([res[c]["y_own"][:TP] for c in range(NCORES)], 0).reshape(1, 8192, D)
    y_s = np.concatenate([res[c]["y_own"][TP:TO] for c in range(NCORES)], 0).reshape(128, 4, D)
    kvp_all = [np.concatenate([res[c][f"kvp{g}"] for c in range(NCORES)], 0) for g in range(3)]
    kv_p = [np.ascontiguousarray(kvp_all[g][-W:]).reshape(1, 1, W, 2, 4, 128) for g, W in enumerate((128, 512, 2048))]
    sp = res[0]["ssm_p"]
    ssm_re_p = np.ascontiguousarray(sp[:, :64]).reshape(1, 1, 64, 64)
    ssm_im_p = np.ascontiguousarray(sp[:, 64:]).reshape(1, 1, 64, 64)
    kv_s = [np.concatenate([res[c][f"kvs{g}"] for c in range(NCORES)], 0).reshape(1, 128, 4, 2, 4, 128) for g in range(3)]
    ss = np.concatenate([res[c]["ssm_s"][3::4] for c in range(NCORES)], 0).reshape(128, 64, 2, 64)
    ssm_re_s = np.ascontiguousarray(ss[:, :, 0]).reshape(1, 128, 64, 64)
    ssm_im_s = np.ascontiguousarray(ss[:, :, 1]).reshape(1, 128, 64, 64)
    outs = (y_p, y_s, kv_p[0], kv_p[1], kv_p[2], ssm_re_p, ssm_im_p, kv_s[0], kv_s[1], kv_s[2], ssm_re_s, ssm_im_s)
    return tuple(np.ascontiguousarray(o, dtype=f32) for o in outs)


KSTAGE = 5


def kernel(**inputs):
    res = run(inputs, KSTAGE)
    return assemble(res)
```
